# Optimizing a Trainium2 kernel written in Bass

```python
import jax, jax.numpy as jnp
from jax import lax
import numpy as np

D_MODEL = 1024
BATCH = 8
SEQ = 4096
DEPTH = 2

D_MIX = D_MODEL
N_ATTN_HEADS = 8
HEAD_DIM = 64
D_ATTN = N_ATTN_HEADS * HEAD_DIM
D_LRU = D_MIX - D_ATTN
N_LRU_BLOCKS = 8
LRU_BLOCK = D_LRU // N_LRU_BLOCKS
CONV_WIDTH = 4
LRU_C = 8.0
DILATED_PATTERNS = ((128, 1), (512, 4), (2048, 16))
N_BUCKETS = 32
MAX_DISTANCE = 2048
D_IN = 3 * D_ATTN + 2 * D_LRU
D_FF_DENSE = 2816
N_EXPERTS = 8
TOP_K = 2
D_FF_EXPERT = 3584
N_DENSE = (DEPTH + 1) // 2
N_MOE = DEPTH // 2
DEEPNORM_ALPHA = (2.0 * DEPTH) ** 0.25
DEEPNORM_BETA = (8.0 * DEPTH) ** -0.25
LN_EPS = 1e-5
RMS_EPS = 1e-6
NEG_INF = -1e30

kernel_name = 'hybrid_rglru_dilated_attn_moe'

f32 = jnp.float32


def _t5_bucket(dist):
    max_exact = N_BUCKETS // 2
    d = np.maximum(dist, 1).astype(np.float32)
    large = max_exact + (np.log(d / max_exact) / np.log(MAX_DISTANCE / max_exact)
                         * (N_BUCKETS - max_exact)).astype(np.int32)
    large = np.minimum(large, N_BUCKETS - 1)
    return np.where(dist < max_exact, dist, large).astype(np.int32)


def _dilated_window_attention(q, k, v, rel_bias, window, dilation):
    B, S, H, Dh = q.shape
    nk = window // dilation
    L = S // dilation
    nb = -(-L // nk)
    Lp = nb * nk

    def by_residue(t):
        t = t.reshape(B, L, dilation, H, Dh).transpose(0, 2, 1, 3, 4)
        return jnp.pad(t, ((0, 0), (0, 0), (0, Lp - L), (0, 0), (0, 0)))

    def kv_windows(t):
        t = jnp.pad(by_residue(t), ((0, 0), (0, 0), (nk, 0), (0, 0), (0, 0)))
        t = t.reshape(B, dilation, nb + 1, nk, H, Dh)
        return jnp.concatenate([t[:, :, :-1], t[:, :, 1:]], axis=3)

    qb = by_residue(q).reshape(B, dilation, nb, nk, H, Dh)
    kw = kv_windows(k)
    vw = kv_windows(v)

    qi = np.arange(nk)[:, None]
    kj = np.arange(2 * nk)[None, :]
    delta = qi + nk - kj
    band = (delta >= 0) & (delta <= nk)
    nonneg = (np.arange(nb)[:, None, None] > 0) | (kj >= nk)[None]
    valid = jnp.asarray(band[None] & nonneg)
    bucket = _t5_bucket(np.clip(delta, 0, nk) * dilation)
    bias = jnp.transpose(rel_bias.astype(f32)[bucket], (2, 0, 1))

    s = jnp.einsum('brnqhc,brnkhc->brnhqk', qb.astype(f32), kw.astype(f32)) * (HEAD_DIM ** -0.5) + bias
    s = jnp.where(valid[:, None], s, NEG_INF)
    lse = jax.nn.logsumexp(s, axis=-1)
    p = jnp.exp(s - lse[..., None])
    o = jnp.einsum('brnhqk,brnkhc->brnqhc', p.astype(v.dtype), vw)

    def back(t):
        t = jnp.swapaxes(t[:, :, :L], 1, 2)
        return t.reshape((B, S) + t.shape[3:])

    o = back(o.reshape(B, dilation, Lp, H, Dh))
    lse = back(jnp.swapaxes(lse, 3, 4).reshape(B, dilation, Lp, H))
    return o, lse


def _mixture_of_dilations(q, k, v, rel_bias):
    outs, lses = [], []
    for window, dilation in DILATED_PATTERNS:
        o, l = _dilated_window_attention(q, k, v, rel_bias, window, dilation)
        outs.append(o.astype(f32))
        lses.append(l)
    w = jax.nn.softmax(jnp.stack(lses), axis=0)
    return jnp.einsum('pbsh,pbshc->bshc', w, jnp.stack(outs))


def _rglru_branch(u, gate, conv_w, conv_b, w_a, b_a, w_x, b_x, lru_lambda):
    B, S, C = u.shape
    u = lax.conv_general_dilated(u, conv_w, window_strides=(1,), padding=[(CONV_WIDTH - 1, 0)],
                                 dimension_numbers=('NWC', 'WIO', 'NWC'),
                                 feature_group_count=C) + conv_b
    ub = u.reshape(B, S, N_LRU_BLOCKS, LRU_BLOCK)
    r = jax.nn.sigmoid(jnp.einsum('bsgi,gij->bsgj', ub, w_a).reshape(B, S, C) + b_a).astype(f32)
    i = jax.nn.sigmoid(jnp.einsum('bsgi,gij->bsgj', ub, w_x).reshape(B, S, C) + b_x).astype(f32)
    log_a = -LRU_C * r * jax.nn.softplus(-lru_lambda.astype(f32))
    a = jnp.exp(log_a)
    beta = jnp.sqrt(-jnp.expm1(2.0 * log_a)) * (i * u.astype(f32))

    def combine(left, right):
        a_l, b_l = left
        a_r, b_r = right
        return a_l * a_r, a_r * b_l + b_r

    _, h = lax.associative_scan(combine, (a, beta), axis=1)
    return jax.nn.gelu(gate.astype(f32)) * h


def _rms_norm(t, g):
    t = t.astype(f32)
    return t * lax.rsqrt(jnp.mean(t * t, axis=-1, keepdims=True) + RMS_EPS) * g.astype(f32)


def _layer_norm(t, g, b):
    tf = t.astype(f32)
    mu = jnp.mean(tf, axis=-1, keepdims=True)
    var = jnp.mean(jnp.square(tf - mu), axis=-1, keepdims=True)
    y = (tf - mu) * lax.rsqrt(var + LN_EPS) * g.astype(f32) + b.astype(f32)
    return y.astype(t.dtype)


def _hybrid_mixer(x, w_in, conv_w, conv_b, w_a, b_a, w_x, b_x, lru_lambda, rel_bias,
                  g_attn, g_lru, w_out):
    B, S, _ = x.shape
    p = jnp.einsum('bsd,de->bse', x, w_in)
    q, k, v, u, gate = jnp.split(p, [D_ATTN, 2 * D_ATTN, 3 * D_ATTN, 3 * D_ATTN + D_LRU], axis=-1)
    heads = lambda t: t.reshape(B, S, N_ATTN_HEADS, HEAD_DIM)
    attn = _mixture_of_dilations(heads(q), heads(k), heads(v), rel_bias).reshape(B, S, D_ATTN)
    lru = _rglru_branch(u, gate, conv_w, conv_b, w_a, b_a, w_x, b_x, lru_lambda)
    y = jnp.concatenate([_rms_norm(attn, g_attn), _rms_norm(lru, g_lru)], axis=-1).astype(x.dtype)
    return jnp.einsum('bsm,md->bsd', y, w_out)


def _swiglu(x, w_gate, w_up, w_down):
    h = jax.nn.silu(jnp.einsum('bsd,df->bsf', x, w_gate)) * jnp.einsum('bsd,df->bsf', x, w_up)
    return jnp.einsum('bsf,fd->bsd', h, w_down)


def _moe_swiglu(x, router_w, w_gate, w_up, w_down):
    logits = jnp.einsum('bsd,de->bse', x, router_w).astype(f32)
    top_val, top_idx = lax.top_k(logits, TOP_K)
    top_p = jax.nn.softmax(top_val, axis=-1)
    comb = jnp.sum(top_p[..., None] * jax.nn.one_hot(top_idx, N_EXPERTS, dtype=f32), axis=-2)
    y = jnp.zeros(x.shape, f32)
    for e in range(N_EXPERTS):
        y = y + comb[..., e:e + 1] * _swiglu(x, w_gate[e], w_up[e], w_down[e]).astype(f32)
    return y.astype(x.dtype)


def setup_inputs(seed: int = 0) -> dict:
    key = jax.random.key(seed)
    ks = jax.random.split(key, 24)
    nrm = lambda k, shape, scale: jax.random.normal(k, shape, f32) * scale
    a0 = jax.random.uniform(ks[9], (DEPTH, D_LRU), f32, minval=0.9, maxval=0.999)
    s0 = a0 ** (1.0 / LRU_C)
    return {
        'x': nrm(ks[0], (BATCH, SEQ, D_MODEL), 1.0),
        'w_in': nrm(ks[1], (DEPTH, D_MODEL, D_IN), D_MODEL ** -0.5),
        'conv_w': nrm(ks[2], (DEPTH, CONV_WIDTH, 1, D_LRU), CONV_WIDTH ** -0.5),
        'conv_b': nrm(ks[3], (DEPTH, D_LRU), 0.01),
        'w_a': nrm(ks[4], (DEPTH, N_LRU_BLOCKS, LRU_BLOCK, LRU_BLOCK), LRU_BLOCK ** -0.5),
        'b_a': nrm(ks[5], (DEPTH, D_LRU), 0.01),
        'w_x': nrm(ks[6], (DEPTH, N_LRU_BLOCKS, LRU_BLOCK, LRU_BLOCK), LRU_BLOCK ** -0.5),
        'b_x': nrm(ks[7], (DEPTH, D_LRU), 0.01),
        'lru_lambda': jnp.log(s0) - jnp.log1p(-s0),
        'rel_bias': nrm(ks[8], (N_BUCKETS, N_ATTN_HEADS), 0.5),
        'g_attn': 1.0 + nrm(ks[10], (DEPTH, D_ATTN), 0.02),
        'g_lru': 1.0 + nrm(ks[11], (DEPTH, D_LRU), 0.02),
        'w_out': nrm(ks[12], (DEPTH, D_MIX, D_MODEL), D_MIX ** -0.5 * DEEPNORM_BETA),
        'ln1_g': 1.0 + nrm(ks[13], (DEPTH, D_MODEL), 0.02),
        'ln1_b': nrm(ks[14], (DEPTH, D_MODEL), 0.01),
        'ln2_g': 1.0 + nrm(ks[15], (DEPTH, D_MODEL), 0.02),
        'ln2_b': nrm(ks[16], (DEPTH, D_MODEL), 0.01),
        'ffn_w_gate': nrm(ks[17], (N_DENSE, D_MODEL, D_FF_DENSE), D_MODEL ** -0.5),
        'ffn_w_up': nrm(ks[18], (N_DENSE, D_MODEL, D_FF_DENSE), D_MODEL ** -0.5),
        'ffn_w_down': nrm(ks[19], (N_DENSE, D_FF_DENSE, D_MODEL), D_FF_DENSE ** -0.5 * DEEPNORM_BETA),
        'router_w': nrm(ks[20], (N_MOE, D_MODEL, N_EXPERTS), D_MODEL ** -0.5),
        'moe_w_gate': nrm(ks[21], (N_MOE, N_EXPERTS, D_MODEL, D_FF_EXPERT), D_MODEL ** -0.5),
        'moe_w_up': nrm(ks[22], (N_MOE, N_EXPERTS, D_MODEL, D_FF_EXPERT), D_MODEL ** -0.5),
        'moe_w_down': nrm(ks[23], (N_MOE, N_EXPERTS, D_FF_EXPERT, D_MODEL), D_FF_EXPERT ** -0.5 * DEEPNORM_BETA),
    }


def reference(x, w_in, conv_w, conv_b, w_a, b_a, w_x, b_x, lru_lambda, rel_bias, g_attn, g_lru,
              w_out, ln1_g, ln1_b, ln2_g, ln2_b, ffn_w_gate, ffn_w_up, ffn_w_down, router_w,
              moe_w_gate, moe_w_up, moe_w_down):
    for layer in range(DEPTH):
        m = _hybrid_mixer(x, w_in[layer], conv_w[layer], conv_b[layer], w_a[layer], b_a[layer],
                          w_x[layer], b_x[layer], lru_lambda[layer], rel_bias,
                          g_attn[layer], g_lru[layer], w_out[layer])
        x = _layer_norm(DEEPNORM_ALPHA * x + m, ln1_g[layer], ln1_b[layer])
        if layer % 2 == 0:
            j = layer // 2
            f = _swiglu(x, ffn_w_gate[j], ffn_w_up[j], ffn_w_down[j])
        else:
            j = layer // 2
            f = _moe_swiglu(x, router_w[j], moe_w_gate[j], moe_w_up[j], moe_w_down[j])
        x = _layer_norm(DEEPNORM_ALPHA * x + f, ln2_g[layer], ln2_b[layer])
    return x
```

```python
import numpy as np
import concourse.bass as bass
import concourse.mybir as mybir
from concourse.bass_utils import run_bass_kernel_spmd

F32 = mybir.dt.float32
BF16 = mybir.dt.bfloat16
ALU = mybir.AluOpType
AF = mybir.ActivationFunctionType
AX = mybir.AxisListType

NL = 2
SEQ = 4096
DM = 1024
NKC = 8
ALPHA = (2.0 * NL) ** 0.25
LN_EPS = 1e-5
RMS_EPS = 1e-6
PATTERNS = ((128, 1), (512, 4), (2048, 16))
NEG = -30000.0
DFF = 2816
EFF = 3584
NEXP = 8
GC = 2
TP = 512
ST = 512


class G:
    __slots__ = ("w", "r", "name")

    def __init__(self, name=""):
        self.w = None
        self.r = {}
        self.name = name


class Chan:
    def __init__(self, name):
        self.name = name
        self.count = 0
        self.sem = None
        self.last = None


class Op:
    __slots__ = ("eng", "fn", "order", "waits", "sig", "chan", "cnt", "epoch")


class Sched:
    ENGS = ("pe", "act", "dve", "pool", "sp")
    EPOCH_LIMIT = 20000

    def __init__(self):
        self.ops = {e: [] for e in self.ENGS}
        self.chans = []

    def chan(self, name):
        c = Chan(name)
        self.chans.append(c)
        return c

    def add(self, eng, fn, reads=(), writes=(), chan=None):
        op = Op()
        op.eng = eng
        op.fn = fn
        op.chan = chan
        op.sig = False
        op.cnt = None
        op.epoch = None
        op.order = len(self.ops[eng])
        if chan is not None:
            chan.count += 1
            op.cnt = chan.count
            chan.last = op
        need = {}

        def consider(d, raw):
            if d is op:
                return
            if d.chan is None and d.eng == eng:
                if (not raw) or eng == "pe":
                    return
            key = ("c", id(d.chan)) if d.chan is not None else ("e", d.eng)
            prev = need.get(key)
            if prev is None:
                need[key] = d
            else:
                a = d.cnt if d.chan is not None else d.order
                b = prev.cnt if prev.chan is not None else prev.order
                if a > b:
                    need[key] = d

        for t in reads:
            if t.w is not None:
                consider(t.w, True)
        for t in writes:
            if t.w is not None:
                consider(t.w, False)
            for r in t.r.values():
                consider(r, False)
        op.waits = list(need.values())
        for d in op.waits:
            d.sig = True
        for t in writes:
            t.w = op
            t.r = {}
        rk = ("c", id(chan)) if chan is not None else ("e", eng)
        for t in reads:
            if t.w is not op:
                t.r[rk] = op
        self.ops[eng].append(op)
        return op

    def barrier(self):
        gs = []
        for e in self.ENGS:
            real = [o for o in self.ops[e][-64:] if o.fn is not None] or [o for o in self.ops[e] if o.fn is not None]
            if real:
                g = G("bar")
                g.w = real[-1]
                gs.append(g)
        for c in self.chans:
            if c.last is not None:
                g = G("barc")
                g.w = c.last
                gs.append(g)
        for e in self.ENGS:
            self.add(e, None, reads=gs)

    def emit(self, nc, final_eng="sp"):
        nep = {}
        for e in self.ENGS:
            ep = 0
            c = 0
            for op in self.ops[e]:
                if op.chan is None and op.sig:
                    if c >= self.EPOCH_LIMIT:
                        ep += 1
                        c = 0
                    c += 1
                    op.cnt = c
                    op.epoch = ep
            nep[e] = ep + 1
        sems = {}
        for e in self.ENGS:
            for ep in range(nep[e]):
                sems[(e, ep)] = nc.alloc_semaphore(f"s_{e}_{ep}")
        for i, c in enumerate(self.chans):
            c.sem = nc.alloc_semaphore(f"c_{i}_{c.name}")
        self.nsem = len(sems) + len(self.chans)
        engobj = {"pe": "tensor", "act": "scalar", "dve": "vector", "pool": "gpsimd", "sp": "sync"}

        def run_engine(e, eng):
            seen = {}
            for op in self.ops[e]:
                for d in op.waits:
                    if d.chan is not None:
                        sem, val = d.chan.sem, 16 * d.cnt
                    else:
                        sem, val = sems[(d.eng, d.epoch)], d.cnt
                    k = id(sem)
                    if seen.get(k, 0) >= val:
                        continue
                    seen[k] = val
                    eng.wait_ge(sem, val)
                if op.fn is None:
                    continue
                ins = op.fn(eng)
                if op.chan is not None:
                    ins.then_inc(op.chan.sem, 16)
                elif op.sig:
                    ins.then_inc(sems[(e, op.epoch)], 1)
            if e == final_eng:
                for c in self.chans:
                    if c.count > 0:
                        k = id(c.sem)
                        if seen.get(k, 0) < 16 * c.count:
                            eng.wait_ge(c.sem, 16 * c.count)

        with nc.Block() as block:
            for e in self.ENGS:
                deco = getattr(block, engobj[e])

                def mk(e):
                    def f(eng):
                        run_engine(e, eng)

                    return f

                deco(mk(e))


def _t5_bucket(dist):
    n_buckets, max_distance = 32, 2048
    max_exact = n_buckets // 2
    d = np.maximum(dist, 1).astype(np.float32)
    large = max_exact + (np.log(d / max_exact) / np.log(max_distance / max_exact) * (n_buckets - max_exact)).astype(np.int32)
    large = np.minimum(large, n_buckets - 1)
    return np.where(dist < max_exact, dist, large).astype(np.int32)


PP_LRU = 0
PP_GMIX = 64
PP_LN = 80
NPP = 80 + 64


def _prep_shared(inp):
    f = np.float32
    sh = {}
    w_in = inp["w_in"].astype(f, copy=False)
    sh["w_in_r"] = np.ascontiguousarray(w_in.reshape(NL, NKC, 128, 20, 128).transpose(0, 3, 2, 1, 4))
    w_out = inp["w_out"].astype(f, copy=False)
    sh["w_out_r"] = np.ascontiguousarray(w_out.reshape(NL, NKC, 128, DM).transpose(0, 2, 1, 3))
    wg = np.zeros((NL, 2, 4, 128, 128), f)
    for which, name in enumerate(("w_a", "w_x")):
        w = inp[name]
        for c in range(4):
            wg[:, which, c, 0:64, 0:64] = w[:, 2 * c]
            wg[:, which, c, 64:128, 64:128] = w[:, 2 * c + 1]
    sh["wgate_r"] = np.ascontiguousarray(wg.transpose(3, 0, 1, 2, 4).reshape(128, 16, 128))
    pp = np.zeros((128, NPP), f)
    for l in range(NL):
        for c in range(4):
            base = PP_LRU + (l * 4 + c) * 8
            sl = slice(c * 128, (c + 1) * 128)
            for j in range(4):
                pp[:, base + j] = inp["conv_w"][l, j, 0, sl]
            pp[:, base + 4] = inp["conv_b"][l, sl]
            pp[:, base + 5] = inp["b_a"][l, sl]
            pp[:, base + 6] = inp["b_x"][l, sl]
            pp[:, base + 7] = inp["lru_lambda"][l, sl]
        for c in range(4):
            pp[:, PP_GMIX + l * 8 + c] = inp["g_attn"][l, c * 128:(c + 1) * 128]
            pp[:, PP_GMIX + l * 8 + 4 + c] = inp["g_lru"][l, c * 128:(c + 1) * 128]
        for k, name in enumerate(("ln1_g", "ln1_b", "ln2_g", "ln2_b")):
            for c in range(8):
                pp[:, PP_LN + (l * 4 + k) * 8 + c] = inp[name][l, c * 128:(c + 1) * 128]
    sh["pp"] = pp
    rel = inp["rel_bias"].astype(f, copy=False)
    tab = np.full((128, 3, 8, 256), NEG, f)
    kk = np.arange(128)[:, None]
    qq = np.arange(128)[None, :]
    for pi, (window, dil) in enumerate(PATTERNS):
        dc = qq - kk
        bc = _t5_bucket(np.clip(dc, 0, 128) * dil)
        dp = qq + 128 - kk
        bp = _t5_bucket(np.clip(dp, 0, 128) * dil)
        for h in range(8):
            cur = np.where(dc >= 0, rel[bc, h], f(NEG))
            prv = np.where(dp <= 128, rel[bp, h], f(NEG))
            tab[:, pi, h, 0:128] = cur
            tab[:, pi, h, 128:256] = prv
    sh["tab"] = tab
    W = GC * 128
    ng = DFF // W
    sh["fg_r"] = np.ascontiguousarray(inp["ffn_w_gate"][0].reshape(NKC, 128, ng, W).transpose(2, 1, 0, 3))
    sh["fu_r"] = np.ascontiguousarray(inp["ffn_w_up"][0].reshape(NKC, 128, ng, W).transpose(2, 1, 0, 3))
    sh["fd_r"] = np.ascontiguousarray(inp["ffn_w_down"][0].reshape(ng, GC, 128, DM).transpose(0, 2, 1, 3))
    ng = EFF // W
    sh["mg_r"] = np.ascontiguousarray(inp["moe_w_gate"][0].reshape(NEXP, NKC, 128, ng, W).transpose(0, 3, 2, 1, 4))
    sh["mu_r"] = np.ascontiguousarray(inp["moe_w_up"][0].reshape(NEXP, NKC, 128, ng, W).transpose(0, 3, 2, 1, 4))
    sh["md_r"] = np.ascontiguousarray(inp["moe_w_down"][0].reshape(NEXP, ng, GC, 128, DM).transpose(0, 1, 3, 2, 4))
    sh["router_r"] = np.ascontiguousarray(inp["router_w"][0].reshape(NKC, 128, NEXP).transpose(1, 0, 2))
    sel = np.zeros((8, 8, 128), f)
    for e in range(8):
        sel[e, e, :] = 1.0
    sh["sel"] = sel
    sh["ltri"] = np.triu(np.ones((128, 128), f), 1)
    sh["iotap"] = np.arange(128, dtype=f).reshape(128, 1)
    l2 = np.stack([inp["ln2_g"][NL - 1], inp["ln2_b"][NL - 1]], 0).astype(f)
    sh["ln2row"] = np.ascontiguousarray(np.broadcast_to(l2[None], (128, 2, DM)))
    return sh


class SB:
    def __init__(self, nc):
        self.nc = nc
        total = int(nc.SBUF_PARTITION_SIZE_BYTES)
        self.off = (total - int(nc.sbuf_bytes_remaining) + 255) // 256 * 256
        self.n = 0
        self.limit = 208 * 1024

    regions = None
    ridx = 0

    def alloc(self, shape, dtype, name=None):
        nbytes = int(np.prod(shape[1:])) * (2 if dtype == BF16 else 4)
        self.off = (self.off + 63) // 64 * 64
        if self.regions is not None:
            while self.off + nbytes > self.regions[self.ridx][1]:
                self.ridx += 1
                assert self.ridx < len(self.regions), (name, "SBUF regions exhausted")
                self.off = (self.regions[self.ridx][0] + 63) // 64 * 64
        self.n += 1
        t = self.nc.alloc_sbuf_tensor_at(f"{name or 'b'}_{self.n}", list(shape), dtype, offset=self.off)
        self.off += nbytes
        assert self.off <= self.limit, (name, self.off)
        return t.ap()


def build_program(debug=None, stop=None):
    debug = debug or ()
    nc = bass.Bass("TRN2", target_bir_lowering=False)
    S = Sched()

    def din(name, shape, dt=F32):
        return nc.dram_tensor(name, list(shape), dt, kind="ExternalInput").ap()

    W = GC * 128
    x_d = din("x", [SEQ, DM])
    w_in_d = din("w_in_r", [NL, 20, 128, NKC, 128])
    w_out_d = din("w_out_r", [NL, 128, NKC, DM])
    wgate_d = din("wgate_r", [128, 16, 128])
    pp_d = din("pp", [128, NPP])
    tab_d = din("tab", [128, 3, 8, 256])
    fg_d = din("fg_r", [DFF // W, 128, NKC, W])
    fu_d = din("fu_r", [DFF // W, 128, NKC, W])
    fd_d = din("fd_r", [DFF // W, 128, GC, DM])
    mg_d = din("mg_r", [NEXP, EFF // W, 128, NKC, W])
    mu_d = din("mu_r", [NEXP, EFF // W, 128, NKC, W])
    md_d = din("md_r", [NEXP, EFF // W, 128, GC, DM])
    router_d = din("router_r", [128, NKC, NEXP])
    sel_d = din("sel", [8, 8, 128])
    ltri_d = din("ltri", [128, 128])
    iotap_d = din("iotap", [128, 1])
    ln2row_d = din("ln2row", [128, 2, DM])
    NGE = EFF // W
    NST = NEXP * NGE
    NT = 23
    wsc_gm = nc.dram_tensor("wsc_gm", [NST * 128, NKC * W], BF16).ap()
    wsc_um = nc.dram_tensor("wsc_um", [NST * 128, NKC * W], BF16).ap()
    wsc_dm = nc.dram_tensor("wsc_dm", [NST * 128, GC * DM], BF16).ap()
    xg_d = nc.dram_tensor("xg", [NT * 512, DM], BF16).ap()
    yd_d = nc.dram_tensor("yd", [NT * 512, DM], F32).ap()
    x1f_d = nc.dram_tensor("x1f", [SEQ, DM], F32).ap()
    out_d = nc.dram_tensor("out", [SEQ, DM], F32, kind="ExternalOutput").ap()
    yscr = nc.dram_tensor("yscr", [8, 128, SEQ], BF16).ap()
    xsA = nc.dram_tensor("xsA", [128, NKC, SEQ], BF16).ap()
    xsB = nc.dram_tensor("xsB", [128, NKC, SEQ], BF16).ap()
    dbg = {}
    for name, shape in (("xT", [128, NKC, SEQ]), ("y", [8, 128, SEQ])):
        if ("dbg_" + name) in debug:
            dbg[name] = nc.dram_tensor("dbg_" + name, shape, F32, kind="ExternalOutput").ap()

    dumped = set()

    def dump(name, ap, reads):
        key = "dbg_" + name
        if key not in debug or key in dumped:
            return
        dumped.add(key)
        dt_ = nc.dram_tensor(key, list(ap.shape), ap.dtype, kind="ExternalOutput").ap()
        S.add("sp", lambda e: e.dma_start(out=dt_, in_=ap), reads=reads, chan=c_out)

    sb = SB(nc)
    xT_off = sb.off
    xT = sb.alloc([128, NKC, SEQ], BF16, "xT")
    gxT = [G(f"xT{i}") for i in range(SEQ // 512)]
    identf = sb.alloc([128, 128], F32, "identf")
    identb = sb.alloc([128, 128], BF16, "identb")
    onesb = sb.alloc([128, 128], BF16, "onesb")
    pp = sb.alloc([128, NPP], F32, "pp")
    ppx = sb.alloc([128, 64], F32, "ppx")
    wgate = sb.alloc([128, 16, 128], BF16, "wgate")
    routerb = sb.alloc([128, NKC, NEXP], F32, "routerb")
    g_const = G("const")
    g_ppx = G("ppx")
    tmp8 = sb.alloc([128, 8], F32, "tmp8")
    tmp8b = sb.alloc([128, 8], F32, "tmp8b")
    ltri = sb.alloc([128, 128], F32, "ltri")
    onesf = sb.alloc([128, 128], F32, "onesf")
    iotap = sb.alloc([128, 1], F32, "iotap")
    cb_off = sb.off
    NCB = 3
    cbuf = [sb.alloc([128, NKC * W], BF16, f"cbuf{i}") for i in range(NCB)]
    gcb = [G() for _ in range(NCB)]
    arena0 = sb.off

    psum = [nc.alloc_psum_tensor(f"ps{i}", [128, 512], F32).ap() for i in range(8)]
    gps = [G(f"ps{i}") for i in range(8)]

    c_const = S.chan("const")
    c_out = S.chan("out")

    def xg(t0, t1):
        return gxT[t0 // 512:(t1 + 511) // 512]

    S.add("sp", lambda e: e.dma_start(out=pp, in_=pp_d), writes=[g_const], chan=c_const)
    S.add("sp", lambda e: e.dma_start(out=routerb, in_=router_d), writes=[g_const], chan=c_const)
    c_const_sw = S.chan("const_sw")
    S.add("pool", lambda e: e.dma_start(out=wgate, in_=wgate_d), writes=[g_const], chan=c_const_sw)
    S.add("sp", lambda e: e.dma_start(out=ltri, in_=ltri_d), writes=[g_const], chan=c_const)
    S.add("sp", lambda e: e.dma_start(out=iotap, in_=iotap_d), writes=[g_const], chan=c_const)
    g_id = G("ident")
    ccl = [S.chan(f"cvl{i}") for i in range(NCB)]
    ccs = [S.chan(f"cvs{i}") for i in range(NCB)]

    NSD = DFF // W
    wsc_g0 = nc.dram_tensor("wsc_g0", [NSD, 128, NKC, W], BF16).ap()
    wsc_u0 = nc.dram_tensor("wsc_u0", [NSD, 128, NKC, W], BF16).ap()
    wsc_d0 = nc.dram_tensor("wsc_d0", [NSD, 128, GC, DM], BF16).ap()

    def conv_gen():
        k = 0
        for ex in range(NEXP):
            for gi in range(NGE):
                i = ex * NGE + gi
                for src, dst, a in ((mg_d[ex, gi], wsc_gm, NKC), (mu_d[ex, gi], wsc_um, NKC), (md_d[ex, gi], wsc_dm, GC)):
                    b = k % NCB
                    k += 1
                    S.add("pool", lambda e, b=b, src=src, a=a: e.dma_start(out=cbuf[b].rearrange("p (a b) -> p a b", a=a), in_=src), writes=[gcb[b]], chan=ccl[b])
                    S.add("sp", lambda e, b=b, dst=dst, i=i: e.dma_start(out=dst[i * 128:(i + 1) * 128, :], in_=cbuf[b]), reads=[gcb[b]], chan=ccs[b])
                    yield

    conv = conv_gen()

    def conv_pull(n):
        for _ in range(n):
            if next(conv, "done") == "done":
                return

    g_idm = G("identm")

    def mk_ones(e):
        e.memset(onesf, 1.0)
        return e.memset(onesb, 1.0)

    S.add("pool", mk_ones, writes=[g_id])
    S.add("pool", lambda e: e.memset(identf, 0.0), writes=[g_idm])
    S.add("pool", lambda e: e.affine_select(out=identf, in_=identf, pattern=[[-1, 128]], compare_op=ALU.not_equal, fill=1.0, base=0, channel_multiplier=1), reads=[g_idm], writes=[g_id])
    S.add("dve", lambda e: e.tensor_copy(out=identb, in_=identf), reads=[g_id], writes=[g_const])
    g_t1 = G("t1")
    lam = pp.rearrange("p (a b) -> p a b", b=8)[:, 0:8, 7]
    S.add("act", lambda e: e.activation(out=tmp8, in_=lam, func=AF.Exp, scale=-1.0), reads=[g_const], writes=[g_t1])
    g_t2 = G("t2")
    S.add("act", lambda e: e.activation(out=tmp8b, in_=tmp8, func=AF.Ln, bias=1.0, scale=1.0), reads=[g_t1], writes=[g_t2])
    S.add("dve", lambda e: e.tensor_scalar(out=ppx[:, 0:8], in0=tmp8b, scalar1=-8.0, scalar2=None, op0=ALU.mult), reads=[g_t2], writes=[g_ppx])
    S.add("dve", lambda e: e.tensor_scalar(out=ppx[:, 16:16 + 32], in0=pp[:, PP_LN:PP_LN + 32], scalar1=ALPHA, scalar2=None, op0=ALU.mult), reads=[g_const], writes=[g_ppx])
    S.add("dve", lambda e: e.tensor_scalar(out=ppx[:, 32:48], in0=pp[:, PP_LN + 32:PP_LN + 48], scalar1=ALPHA, scalar2=None, op0=ALU.mult), reads=[g_const], writes=[g_ppx])

    def pcol(i):
        return pp[:, i:i + 1]

    sb.off = arena0
    xs = [sb.alloc([128, DM], F32, f"xs{i}") for i in range(2)]
    gxs = [G(f"xs{i}") for i in range(2)]
    cxs = [S.chan(f"xs{i}") for i in range(2)]
    zt = sb.alloc([128, 4, DM], BF16, "zt")
    gzt = G()
    czero = S.chan("zero")
    S.add("pool", lambda e: e.memset(zt, 0.0), writes=[gzt])
    for j in range(NT):
        S.add("sp", lambda e, j=j: e.dma_start(out=xg_d[j * 512:(j + 1) * 512, :].rearrange("(c p) d -> p c d", p=128), in_=zt), reads=[gzt], chan=czero)
    for tt in range(SEQ // 128):
        s = tt % 2
        S.add("sp", lambda e, tt=tt, s=s: e.dma_start(out=xs[s], in_=x_d[tt * 128:(tt + 1) * 128, :]), writes=[gxs[s]], chan=cxs[s])
        for b in range(2):
            bank = (tt % 2) * 2 + b

            def tr(e, s=s, b=b, bank=bank):
                ins = None
                for j in range(4):
                    kc = b * 4 + j
                    ins = e.transpose(psum[bank][:, j * 128:(j + 1) * 128], xs[s][:, kc * 128:(kc + 1) * 128], identf)
                return ins

            S.add("pe", tr, reads=[gxs[s], g_id], writes=[gps[bank]])
            dst = xT[:, b * 4:(b + 1) * 4, tt * 128:(tt + 1) * 128]
            src = psum[bank].rearrange("p (j t) -> p j t", j=4)
            if b == 0:
                S.add("act", lambda e, dst=dst, src=src: e.copy(out=dst, in_=src), reads=[gps[bank]], writes=xg(tt * 128, tt * 128 + 128))
            else:
                S.add("dve", lambda e, dst=dst, src=src: e.tensor_copy(out=dst, in_=src), reads=[gps[bank]], writes=xg(tt * 128, tt * 128 + 128))

    def dump_xT():
        if "xT" in dbg:
            S.barrier()
            sb.off = arena0
            tf = sb.alloc([128, NKC, 512], F32, "dbgt")
            gt = G("dbgt")
            for t in range(8):
                S.add("dve", lambda e, t=t: e.tensor_copy(out=tf, in_=xT[:, :, t * 512:(t + 1) * 512]), reads=[gxT[t]], writes=[gt])
                S.add("sp", lambda e, t=t: e.dma_start(out=dbg["xT"][:, :, t * 512:(t + 1) * 512], in_=tf), reads=[gt], chan=c_out)
            S.barrier()

    if stop == "P1":
        dump_xT()
        S.emit(nc)
        return nc

    def layer_body(layer):
        S.barrier()
        sb.regions = None
        sb.off = arena0
        wlu = [sb.alloc([128, NKC, 256], BF16, f"wlu{i}") for i in range(2)]
        gwlu = [G() for _ in range(2)]
        cwlu = [S.chan(f"wlu{i}") for i in range(2)]
        ufull = sb.alloc([128, 3 + SEQ], F32, "ufull")
        gu = [G() for _ in range(8)]
        gupad = G()
        ych = [sb.alloc([128, SEQ], BF16, f"ych{i}") for i in range(2)]
        gych = [G() for _ in range(2)]
        cych = [S.chan(f"ych{i}") for i in range(2)]
        NB = 3

        def bufs(name, dt=F32, n=NB, w=512):
            return [sb.alloc([128, w], dt, f"{name}{i}") for i in range(n)], [G(name) for _ in range(n)]

        gl, ggl = bufs("gl")
        uc, guc = bufs("uc")
        ucb, gucb = bufs("ucb", BF16)
        rr, grr = bufs("rr")
        ii, gii = bufs("ii")
        aa, gaa = bufs("aa")
        a2, ga2 = bufs("a2")
        bb, gbb = bufs("bb")
        hh, ghh = bufs("hh")
        S.add("pool", lambda e: e.memset(ufull[:, 0:3], 0.0), writes=[gupad])
        def mk_iter(c, tt, it):
            ws = c % 2
            pb = PP_LRU + (layer * 4 + c) * 8
            ys = c % 2
            s = it % NB
            sp_ = (it - 1) % NB
            t0 = tt * 512
            bu, bg = (0, 1) if tt % 2 == 0 else (2, 3)
            br, bi = (4, 5) if tt % 2 == 0 else (6, 7)
            ia = (layer * 2 + 0) * 4 + c
            ix = (layer * 2 + 1) * 4 + c
            cc = layer * 4 + c

            def stage_a():
                if tt == 0:
                    S.add("pool", lambda e: e.dma_start(out=wlu[ws][:, :, 0:128], in_=w_in_d[layer, 12 + c]), writes=[gwlu[ws]], chan=cwlu[ws])
                    S.add("pool", lambda e: e.dma_start(out=wlu[ws][:, :, 128:256], in_=w_in_d[layer, 16 + c]), writes=[gwlu[ws]], chan=cwlu[ws])

                def mm_u(e):
                    ins = None
                    for kc in range(NKC):
                        ins = e.matmul(psum[bu], lhsT=wlu[ws][:, kc, 0:128], rhs=xT[:, kc, t0:t0 + 512], start=(kc == 0), stop=(kc == NKC - 1))
                    return ins

                def mm_g(e):
                    ins = None
                    for kc in range(NKC):
                        ins = e.matmul(psum[bg], lhsT=wlu[ws][:, kc, 128:256], rhs=xT[:, kc, t0:t0 + 512], start=(kc == 0), stop=(kc == NKC - 1))
                    return ins

                S.add("pe", mm_u, reads=[gwlu[ws], gxT[tt]], writes=[gps[bu]])
                S.add("pe", mm_g, reads=[gwlu[ws], gxT[tt]], writes=[gps[bg]])
                S.add("dve", lambda e: e.tensor_copy(out=ufull[:, 3 + t0:3 + t0 + 512], in_=psum[bu]), reads=[gps[bu]], writes=[gu[tt]])
                S.add("act", lambda e: e.activation(out=gl[s], in_=psum[bg], func=AF.Gelu_apprx_tanh), reads=[gps[bg]], writes=[ggl[s]])
                rd = [gu[tt], gupad, g_const] + ([gu[tt - 1]] if tt > 0 else [])
                S.add("pool", lambda e: e.tensor_scalar(out=uc[s], in0=ufull[:, t0:t0 + 512], scalar1=pcol(pb), scalar2=pcol(pb + 4), op0=ALU.mult, op1=ALU.add), reads=rd, writes=[guc[s]])
                for j in range(1, 4):
                    S.add("dve", lambda e, j=j: e.scalar_tensor_tensor(out=uc[s], in0=ufull[:, t0 + j:t0 + j + 512], scalar=pcol(pb + j), in1=uc[s], op0=ALU.mult, op1=ALU.add), reads=rd + [guc[s]], writes=[guc[s]])
                S.add("act", lambda e: e.copy(out=ucb[s], in_=uc[s]), reads=[guc[s]], writes=[gucb[s]])

            def stage_b():
                S.add("pe", lambda e: e.matmul(psum[br], lhsT=wgate[:, ia, :], rhs=ucb[s], start=True, stop=True), reads=[gucb[s], g_const], writes=[gps[br]])
                S.add("pe", lambda e: e.matmul(psum[bi], lhsT=wgate[:, ix, :], rhs=ucb[s], start=True, stop=True), reads=[gucb[s], g_const], writes=[gps[bi]])
                S.add("act", lambda e: e.activation(out=rr[s], in_=psum[br], func=AF.Sigmoid, bias=pcol(pb + 5), scale=1.0), reads=[gps[br], g_const], writes=[grr[s]])
                S.add("act", lambda e: e.activation(out=ii[s], in_=psum[bi], func=AF.Sigmoid, bias=pcol(pb + 6), scale=1.0), reads=[gps[bi], g_const], writes=[gii[s]])
                S.add("act", lambda e: e.activation(out=aa[s], in_=rr[s], func=AF.Exp, scale=ppx[:, cc:cc + 1]), reads=[grr[s], g_ppx], writes=[gaa[s]])
                S.add("dve", lambda e: e.tensor_tensor(out=a2[s], in0=aa[s], in1=aa[s], op=ALU.mult), reads=[gaa[s]], writes=[ga2[s]])
                S.add("act", lambda e: e.activation(out=a2[s], in_=a2[s], func=AF.Sqrt, scale=-1.0, bias=1.0), reads=[ga2[s]], writes=[ga2[s]])
                S.add("dve", lambda e: e.tensor_tensor(out=bb[s], in0=ii[s], in1=uc[s], op=ALU.mult), reads=[gii[s], guc[s]], writes=[gbb[s]])
                S.add("dve", lambda e: e.tensor_tensor(out=bb[s], in0=bb[s], in1=a2[s], op=ALU.mult), reads=[gbb[s], ga2[s]], writes=[gbb[s]])

            def stage_c():
                if tt == 0:
                    S.add("dve", lambda e: e.tensor_tensor_scan(out=hh[s], data0=aa[s], data1=bb[s], initial=0.0, op0=ALU.mult, op1=ALU.add), reads=[gaa[s], gbb[s]], writes=[ghh[s]])
                else:
                    S.add("dve", lambda e: e.tensor_tensor_scan(out=hh[s], data0=aa[s], data1=bb[s], initial=hh[sp_][:, 511:512], op0=ALU.mult, op1=ALU.add), reads=[gaa[s], gbb[s], ghh[sp_]], writes=[ghh[s]])
                S.add("pool", lambda e: e.tensor_tensor(out=ych[ys][:, t0:t0 + 512], in0=gl[s], in1=hh[s], op=ALU.mult), reads=[ggl[s], ghh[s]], writes=[gych[ys]])
                if tt == 7:
                    S.add("sp", lambda e: e.dma_start(out=yscr[4 + c], in_=ych[ys]), reads=[gych[ys]], chan=cych[ys])

            return stage_a, stage_b, stage_c

        iters = [mk_iter(c, tt, c * 8 + tt) for c in range(4) for tt in range(8)]
        NI = len(iters)
        for k in range(NI + 2):
            if k < NI:
                iters[k][0]()
            if 0 <= k - 1 < NI:
                iters[k - 1][1]()
            if 0 <= k - 2 < NI:
                iters[k - 2][2]()

        if stop == f"LRU{layer}":
            return True
        S.barrier()
        sb.off = arena0
        tab = sb.alloc([128, 3, 8, 256], BF16, "tab")
        gtab = G()
        gtst = G()
        wq = [sb.alloc([128, NKC, 384], BF16, f"wq{i}") for i in range(2)]
        gwq = [G() for _ in range(2)]
        cwq = [S.chan(f"wq{i}") for i in range(2)]
        qT = sb.alloc([128, SEQ], BF16, "qT")
        kT = sb.alloc([128, SEQ], BF16, "kT")
        gq, gk = G(), G()
        vt = [sb.alloc([128, 32, 128], BF16, f"vt{i}") for i in range(2)]
        gvt = [G() for _ in range(2)]
        acc = sb.alloc([128, SEQ], F32, "acc")
        den = sb.alloc([128, SEQ], F32, "den")
        gacc, gden = G(), G()
        tstage = acc[:, 0:1024].rearrange("p (a b) -> p a b", a=4)
        vT = sb.alloc([128, SEQ], BF16, "vT")
        gv = G()
        pT = [sb.alloc([128, 256], BF16, f"pT{i}") for i in range(4)]
        gpT = [G() for _ in range(4)]
        ych = [sb.alloc([128, SEQ], BF16, "ycha0")] * 2
        gych = [G()] * 2
        cych = [S.chan("ycha0")] * 2
        ctab = S.chan("tab")
        for pi_ in range(3):
            for hq in range(2):
                S.add("sp", lambda e, pi_=pi_, hq=hq: e.dma_start(out=tstage, in_=tab_d[:, pi_, hq * 4:(hq + 1) * 4, :]), writes=[gtst], chan=ctab)
                S.add("act", lambda e, pi_=pi_, hq=hq: e.activation(out=tab[:, pi_, hq * 4:(hq + 1) * 4, :], in_=tstage, func=AF.Exp), reads=[gtst], writes=[gtab])
        vcount = 0
        pslot = 0
        GS = [G() for _ in range(4)]
        for hp in range(4):
            ws = hp % 2
            for j, gidx in enumerate((hp, 4 + hp, 8 + hp)):
                S.add("pool", lambda e, ws=ws, j=j, gidx=gidx: e.dma_start(out=wq[ws][:, :, j * 128:(j + 1) * 128], in_=w_in_d[layer, gidx]), writes=[gwq[ws]], chan=cwq[ws])
            for tt in range(8):
                t0 = tt * 512
                for which, (dst, gd) in enumerate(((qT, gq), (kT, gk), (vT, gv))):
                    bank = 6 + which % 2

                    def mmq(e, ws=ws, t0=t0, which=which, bank=bank):
                        ins = None
                        for kc in range(NKC):
                            ins = e.matmul(psum[bank], lhsT=wq[ws][:, kc, which * 128:(which + 1) * 128], rhs=xT[:, kc, t0:t0 + 512], start=(kc == 0), stop=(kc == NKC - 1))
                        return ins

                    S.add("pe", mmq, reads=[gwq[ws], gxT[tt]], writes=[gps[bank]])
                    if which == 0:
                        S.add("act", lambda e, t0=t0, bank=bank: e.activation(out=qT[:, t0:t0 + 512], in_=psum[bank], func=AF.Copy, scale=0.125), reads=[gps[bank]], writes=[gq])
                    elif which == 2:
                        S.add("act", lambda e, t0=t0, bank=bank: e.copy(out=vT[:, t0:t0 + 512], in_=psum[bank]), reads=[gps[bank]], writes=[gv])
                    else:
                        S.add("dve", lambda e, t0=t0, bank=bank: e.tensor_copy(out=kT[:, t0:t0 + 512], in_=psum[bank]), reads=[gps[bank]], writes=[gk])
            for pi, (window, dil) in enumerate(PATTERNS):
                nb = SEQ // (128 * dil)
                vs = vcount % 2
                vcount += 1
                for kb4 in range(8):
                    bank = 6 + (kb4 % 2)

                    def mmv(e, ws=ws, kb4=kb4, bank=bank, nb=nb, dil=dil):
                        ins = None
                        for j in range(4):
                            kb = kb4 * 4 + j
                            r, n = kb // nb, kb % nb
                            st = n * 128 * dil + r
                            ins = e.matmul(psum[bank][:, j * 128:(j + 1) * 128], lhsT=vT[:, st:st + 127 * dil + 1:dil], rhs=identb, start=True, stop=True)
                        return ins

                    S.add("pe", mmv, reads=[gv, g_const], writes=[gps[bank]])
                    eng = "act" if kb4 % 2 == 0 else "dve"
                    src = psum[bank].rearrange("p (j c) -> p j c", j=4)
                    dstv = vt[vs][:, kb4 * 4:(kb4 + 1) * 4, :]
                    if eng == "act":
                        S.add("act", lambda e, dstv=dstv, src=src: e.copy(out=dstv, in_=src), reads=[gps[bank]], writes=[gvt[vs]])
                    else:
                        S.add("dve", lambda e, dstv=dstv, src=src: e.tensor_copy(out=dstv, in_=src), reads=[gps[bank]], writes=[gvt[vs]])
                gs = min(4, nb)
                ogrp = 0
                fronts, backs = [], []
                for r in range(dil):
                    for n in range(nb):
                        kb = r * nb + n
                        st = n * 128 * dil + r
                        cnt = 256 if n < nb - 1 else 128
                        grp = n // gs
                        ob = 2 + (ogrp + grp) % 2
                        db = 4 + (ogrp + grp) % 2
                        col = (n % gs) * 128
                        nxt_grp = (n + 1) // gs
                        ob2 = 2 + (ogrp + nxt_grp) % 2
                        db2 = 4 + (ogrp + nxt_grp) % 2
                        col2 = ((n + 1) % gs) * 128
                        for h2 in range(2):
                            h = hp * 2 + h2
                            p0 = 64 * h2
                            sl = pslot % 4
                            pslot += 1
                            sbank = (0, 1, 6, 7)[sl]

                            def front(p0=p0, st=st, cnt=cnt, dil=dil, sbank=sbank, pi=pi, h=h, sl=sl):
                                def mms(e):
                                    return e.matmul(psum[sbank][:, 0:cnt], lhsT=kT[p0:p0 + 64, st:st + 127 * dil + 1:dil], rhs=qT[p0:p0 + 64, st:st + (cnt - 1) * dil + 1:dil], start=True, stop=True)

                                S.add("pe", mms, reads=[gq, gk], writes=[gps[sbank]])
                                S.add("act", lambda e: e.activation(out=pT[sl][:, 0:cnt], in_=psum[sbank][:, 0:cnt], func=AF.Exp), reads=[gps[sbank]], writes=[gpT[sl]])
                                S.add("dve", lambda e: e.tensor_tensor(out=pT[sl][:, 0:cnt], in0=pT[sl][:, 0:cnt], in1=tab[:, pi, h, 0:cnt], op=ALU.mult), reads=[gpT[sl], gtab], writes=[gpT[sl]])

                            def back(sl=sl, vs=vs, kb=kb, p0=p0, n=n, nb=nb, ob=ob, db=db, col=col, ob2=ob2, db2=db2, col2=col2, h2=h2, r=r, dil=dil, gs=gs, pi=pi):
                                def mmo(e):
                                    e.matmul(psum[ob][p0:p0 + 64, col:col + 128], lhsT=vt[vs][:, kb, p0:p0 + 64], rhs=pT[sl][:, 0:128], start=(n == 0), stop=True)
                                    ins = e.matmul(psum[db][p0:p0 + 64, col:col + 128], lhsT=onesb[:, 0:64], rhs=pT[sl][:, 0:128], start=(n == 0), stop=True)
                                    if n < nb - 1:
                                        e.matmul(psum[ob2][p0:p0 + 64, col2:col2 + 128], lhsT=vt[vs][:, kb, p0:p0 + 64], rhs=pT[sl][:, 128:256], start=True, stop=False)
                                        ins = e.matmul(psum[db2][p0:p0 + 64, col2:col2 + 128], lhsT=onesb[:, 0:64], rhs=pT[sl][:, 128:256], start=True, stop=False)
                                    return ins

                                wr = [gps[ob], gps[db]]
                                if n < nb - 1 and ob2 != ob:
                                    wr += [gps[ob2], gps[db2]]
                                S.add("pe", mmo, reads=[gpT[sl], gvt[vs], g_id], writes=wr)
                                if h2 == 1 and n % gs == gs - 1:
                                    g0 = n - (gs - 1)
                                    tst = g0 * 128 * dil + r
                                    ncol = gs * 128
                                    asl = acc[:, tst:tst + (ncol - 1) * dil + 1:dil]
                                    dsl = den[:, tst:tst + (ncol - 1) * dil + 1:dil]
                                    if pi == 0:
                                        S.add("dve", lambda e: e.tensor_copy(out=asl, in_=psum[ob][:, 0:ncol]), reads=[gps[ob]], writes=[gacc])
                                        S.add("dve", lambda e: e.tensor_copy(out=dsl, in_=psum[db][:, 0:ncol]), reads=[gps[db]], writes=[gden])
                                    else:
                                        S.add("dve", lambda e: e.tensor_tensor(out=asl, in0=asl, in1=psum[ob][:, 0:ncol], op=ALU.add), reads=[gps[ob], gacc], writes=[gacc])
                                        S.add("dve", lambda e: e.tensor_tensor(out=dsl, in0=dsl, in1=psum[db][:, 0:ncol], op=ALU.add), reads=[gps[db], gden], writes=[gden])

                            fronts.append(front)
                            backs.append(back)
                    ogrp += (nb + gs - 1) // gs
                LOOK = 3
                for i in range(len(fronts) + LOOK):
                    if i % 5 == 0:
                        conv_pull(1)
                    if i < len(fronts):
                        fronts[i]()
                    if i >= LOOK:
                        backs[i - LOOK]()
            ys = hp % 2
            S.add("dve", lambda e: e.reciprocal(out=den, in_=den), reads=[gden], writes=[gden])
            S.add("pool", lambda e, ys=ys: e.tensor_tensor(out=ych[ys], in0=acc, in1=den, op=ALU.mult), reads=[gacc, gden], writes=[gych[ys]])
            S.add("sp", lambda e, ys=ys, hp=hp: e.dma_start(out=yscr[hp], in_=ych[ys]), reads=[gych[ys]], chan=cych[ys])
        if stop == f"ATT{layer}":
            return True
        S.barrier()
        last = layer == NL - 1
        moe = layer % 2 == 1
        NP = SEQ // TP
        if layer == 0:
            cspill = S.chan("spill")
            for t in range(8):
                S.add("sp", lambda e, t=t: e.dma_start(out=xsA[:, :, t * 512:(t + 1) * 512], in_=xT[:, :, t * 512:(t + 1) * 512]), reads=[gxT[t]], chan=cspill)
            S.barrier()
        res_src = xsA if layer == 0 else xsB
        NTT = SEQ // 128
        if moe:
            assert last
            conv_pull(10 ** 6)
            S.barrier()
            sb.regions = None
            sb.off = xT_off
            x1tok = sb.alloc([128, NTT, DM], BF16, "x1tok")
            gx1tok = [G() for _ in range(NTT)]
            sb.off = cb_off
            lgall = sb.alloc([128, NTT, 8], F32, "lgall")
            glgall = G()
            w1 = sb.alloc([128, NTT], F32, "w1")
            w2 = sb.alloc([128, NTT], F32, "w2")
            slot1i = sb.alloc([128, NTT], mybir.dt.int32, "slot1i")
            slot2i = sb.alloc([128, NTT], mybir.dt.int32, "slot2i")
            widx = sb.alloc([128, NT, NGE], mybir.dt.int32, "widx")
            gw12, gslot, gwidx = G(), G(), G()
            arena1 = sb.off
        else:
            sb.regions = [(xT_off, xT_off + 128 * 0 + NKC * SEQ * 2), (cb_off, sb.limit)]
            sb.ridx = 0
            sb.off = xT_off
        accs = [sb.alloc([128, NKC, TP], F32, f"acc{i}") for i in range(2)]
        gaccs = [[G() for _ in range(NKC)] for _ in range(2)]
        if moe:
            x1bs = [sb.alloc([128, NKC, TP], BF16, "x1b0")] * 2
            gx1bs = [G()] * 2
        else:
            x1bs = [sb.alloc([128, NKC, TP], BF16, f"x1b{i}") for i in range(2)]
            gx1bs = [G() for _ in range(2)]
        ybuf = sb.alloc([128, NKC, TP], BF16, "ybuf")
        gybuf = G()
        cybuf = S.chan("ybuf")
        if moe:
            xres = [sb.alloc([128, NKC, TP], BF16, "xres0")] * 2
            gxres = [G()] * 2
            cxres = [S.chan("xres0")] * 2
        else:
            xres = [sb.alloc([128, NKC, TP], BF16, f"xres{i}") for i in range(2)]
            gxres = [G() for _ in range(2)]
            cxres = [S.chan(f"xres{i}") for i in range(2)]
        if not moe:
            wgs = [sb.alloc([128, NKC, W], BF16, f"wg{i}") for i in range(2)]
            wus = [sb.alloc([128, NKC, W], BF16, f"wu{i}") for i in range(2)]
            wds = [sb.alloc([128, GC, DM], BF16, f"wd{i}") for i in range(2)]
            gwgu = [G() for _ in range(2)]
            gwd = [G() for _ in range(2)]
            cgu = [S.chan(f"wgu{i}") for i in range(2)]
            cdd = [S.chan(f"wdd{i}") for i in range(2)]
            cguh = [S.chan(f"wguh{i}") for i in range(2)]
            cddh = [S.chan(f"wddh{i}") for i in range(2)]
            cwb = [S.chan(f"wwb{i}") for i in range(2)]
            cwbd = [S.chan(f"wwbd{i}") for i in range(2)]
            nst_ = DFF // W
            assert layer == 0
            wsc_g, wsc_u, wsc_d = wsc_g0, wsc_u0, wsc_d0
            gscr = [G() for _ in range(nst_)]
            gscrd = [G() for _ in range(nst_)]
            hT = [sb.alloc([128, GC, TP], BF16, f"hT{i}") for i in range(2)]
            ghT = [G() for _ in range(2)]
            sg = [sb.alloc([128, TP], F32, f"sg{i}") for i in range(2)]
            gsg = [G() for _ in range(2)]
        woutb = sb.alloc([128, NKC, DM], BF16, "woutb")
        gwout = G()
        wstage = [sb.alloc([128, DM], F32, f"wstage{i}") for i in range(2)]
        gwstage = [G() for _ in range(2)]
        cwstage = [S.chan(f"wstage{i}") for i in range(2)]
        sqb = sb.alloc([128, NKC, TP], BF16, "sqb")
        gsqb = G()
        ynb = sb.alloc([128, NKC, TP], BF16, "ynb")
        gynb = G()
        stt = [sb.alloc([128, TP], F32, f"stt{i}") for i in range(3)]
        gstt = [G() for _ in range(3)]
        if last:
            ostage = wstage
            gost = gwstage
            cost = [S.chan(f"ost{i}") for i in range(2)]
        else:
            xo = [sb.alloc([128, NKC, TP], BF16, "xo0")] * 2
            gxo = [G()] * 2
            cxo = [S.chan("xo0")] * 2
        for mc in range(NKC):
            s = mc % 2
            S.add("sp", lambda e, s=s, mc=mc: e.dma_start(out=wstage[s], in_=w_out_d[layer, :, mc, :]), writes=[gwstage[s]], chan=cwstage[s])
            S.add("dve", lambda e, s=s, mc=mc: e.tensor_scalar(out=woutb[:, mc, :], in0=wstage[s], scalar1=pcol(PP_GMIX + layer * 8 + mc), scalar2=None, op0=ALU.mult), reads=[gwstage[s], g_const], writes=[gwout])
        lnb = PP_LN + layer * 32
        bshape = [128, NKC, TP]

        def layer_norm(q, out_fn):
            accb, gacc = accs[q], gaccs[q]
            S.add("act", lambda e: e.copy(out=ynb, in_=accb), reads=gacc, writes=[gynb])
            S.add("pool", lambda e: e.tensor_tensor(out=sqb, in0=accb, in1=accb, op=ALU.mult), reads=gacc, writes=[gsqb])
            yield

            def mm1(e):
                ins = None
                for dc in range(NKC):
                    ins = e.matmul(psum[6], lhsT=onesb, rhs=ynb[:, dc, :], start=(dc == 0), stop=(dc == NKC - 1))
                return ins

            def mm2(e):
                ins = None
                for dc in range(NKC):
                    ins = e.matmul(psum[7], lhsT=onesb, rhs=sqb[:, dc, :], start=(dc == 0), stop=(dc == NKC - 1))
                return ins

            S.add("pe", mm1, reads=[gynb, g_id], writes=[gps[6]])
            S.add("pe", mm2, reads=[gsqb, g_id], writes=[gps[7]])
            yield
            mean, msq, var = stt[0], stt[1], stt[2]
            S.add("act", lambda e: e.activation(out=mean, in_=psum[6], func=AF.Copy, scale=1.0 / DM), reads=[gps[6]], writes=[gstt[0]])
            S.add("dve", lambda e: e.tensor_tensor(out=msq, in0=mean, in1=mean, op=ALU.mult), reads=[gstt[0]], writes=[gstt[1]])
            S.add("dve", lambda e: e.scalar_tensor_tensor(out=var, in0=psum[7], scalar=1.0 / DM, in1=msq, op0=ALU.mult, op1=ALU.subtract), reads=[gps[7], gstt[1]], writes=[gstt[2]])
            S.add("dve", lambda e: e.tensor_scalar(out=var, in0=var, scalar1=LN_EPS, scalar2=None, op0=ALU.add), reads=[gstt[2]], writes=[gstt[2]])
            S.add("act", lambda e: e.activation(out=var, in_=var, func=AF.Sqrt), reads=[gstt[2]], writes=[gstt[2]])
            S.add("dve", lambda e: e.reciprocal(out=var, in_=var), reads=[gstt[2]], writes=[gstt[2]])
            yield
            if moe:
                S.add("dve", lambda e: e.tensor_tensor(out=accb, in0=accb, in1=mean.unsqueeze(1).to_broadcast(bshape), op=ALU.subtract), reads=gacc + [gstt[0]], writes=gacc)
                S.add("dve", lambda e: e.tensor_tensor(out=accb, in0=accb, in1=var.unsqueeze(1).to_broadcast(bshape), op=ALU.mult), reads=gacc + [gstt[2]], writes=gacc)
                yield
            else:
                for dc in range(NKC):
                    S.add("dve", lambda e, dc=dc: e.tensor_tensor(out=accb[:, dc, :], in0=accb[:, dc, :], in1=mean, op=ALU.subtract), reads=[gacc[dc], gstt[0]], writes=[gacc[dc]])
                    S.add("dve", lambda e, dc=dc: e.tensor_tensor(out=accb[:, dc, :], in0=accb[:, dc, :], in1=var, op=ALU.mult), reads=[gacc[dc], gstt[2]], writes=[gacc[dc]])
                    if dc % 2 == 1:
                        yield
            for dc in range(NKC):
                out_fn(dc)
                if dc % 4 == 3:
                    yield

        def phase_a(p):
            q = p % 2
            t0 = p * TP
            accb, gacc, x1b, gx1b = accs[q], gaccs[q], x1bs[q], gx1bs[q]
            S.add("sp", lambda e: e.dma_start(out=xres[q], in_=res_src[:, :, t0:t0 + TP]), writes=[gxres[q]], chan=cxres[q])
            S.add("sp", lambda e: e.dma_start(out=ybuf, in_=yscr[:, :, t0:t0 + TP].rearrange("c p t -> p c t")), writes=[gybuf], chan=cybuf)
            S.add("act", lambda e: e.activation(out=sqb, in_=ybuf, func=AF.Square), reads=[gybuf], writes=[gsqb])
            yield
            for half in range(2):
                def mmss(e, half=half):
                    ins = None
                    for c in range(4):
                        ins = e.matmul(psum[6 + half], lhsT=onesb, rhs=sqb[:, half * 4 + c, :], start=(c == 0), stop=(c == 3))
                    return ins

                S.add("pe", mmss, reads=[gsqb, g_id], writes=[gps[6 + half]])
            yield
            for half in range(2):
                rs = stt[1 + half]
                grs = gstt[1 + half]
                S.add("dve", lambda e, rs=rs, half=half: e.tensor_scalar(out=rs, in0=psum[6 + half], scalar1=1.0 / 512, scalar2=RMS_EPS, op0=ALU.mult, op1=ALU.add), reads=[gps[6 + half]], writes=[grs])
                S.add("act", lambda e, rs=rs: e.activation(out=rs, in_=rs, func=AF.Sqrt), reads=[grs], writes=[grs])
                S.add("dve", lambda e, rs=rs: e.reciprocal(out=rs, in_=rs), reads=[grs], writes=[grs])
                S.add("pool", lambda e, rs=rs, half=half: e.tensor_tensor(out=ynb[:, half * 4:(half + 1) * 4, :], in0=ybuf[:, half * 4:(half + 1) * 4, :], in1=rs.unsqueeze(1).to_broadcast([128, 4, TP]), op=ALU.mult), reads=[gybuf, grs], writes=[gynb])
            yield
            for dc in range(NKC):
                bank = 6 + (dc % 2)

                def mmo_(e, dc=dc, bank=bank):
                    ins = None
                    for mc in range(NKC):
                        ins = e.matmul(psum[bank], lhsT=woutb[:, mc, dc * 128:(dc + 1) * 128], rhs=ynb[:, mc, :], start=(mc == 0), stop=(mc == NKC - 1))
                    return ins

                S.add("pe", mmo_, reads=[gynb, gwout], writes=[gps[bank]])
                S.add("dve", lambda e, dc=dc, bank=bank: e.scalar_tensor_tensor(out=accb[:, dc, :], in0=xres[q][:, dc, :], scalar=ALPHA, in1=psum[bank], op0=ALU.mult, op1=ALU.add), reads=[gps[bank], gxres[q]], writes=[gacc[dc]])
                if dc % 2 == 1:
                    yield

            def ln1_out(dc):
                S.add("act", lambda e, dc=dc: e.activation(out=x1b[:, dc, :], in_=accb[:, dc, :], func=AF.Identity, scale=pcol(lnb + dc), bias=pcol(lnb + 8 + dc)), reads=[gacc[dc], g_const], writes=[gx1b])
                S.add("pool", lambda e, dc=dc: e.tensor_scalar(out=accb[:, dc, :], in0=accb[:, dc, :], scalar1=ppx[:, 16 + layer * 16 + dc:16 + layer * 16 + dc + 1], scalar2=ppx[:, 16 + layer * 16 + 8 + dc:16 + layer * 16 + 8 + dc + 1], op0=ALU.mult, op1=ALU.add), reads=[gacc[dc], g_ppx], writes=[gacc[dc]])

            yield from layer_norm(q, ln1_out)
            if moe:
                def mmr(e):
                    ins = None
                    for tb in range(4):
                        for dc in range(NKC):
                            ins = e.matmul(psum[6][:, tb * 8:(tb + 1) * 8], lhsT=accb[:, dc, tb * 128:(tb + 1) * 128], rhs=routerb[:, dc, :], start=(dc == 0), stop=(dc == NKC - 1))
                    return ins

                S.add("pe", mmr, reads=gacc + [g_const], writes=[gps[6]])
                yield
                S.add("act", lambda e: e.activation(out=lgall[:, p * 4:(p + 1) * 4, :], in_=psum[6][:, 0:32].rearrange("p (a b) -> p a b", a=4), func=AF.Copy, scale=1.0 / ALPHA), reads=[gps[6]], writes=[glgall])
                for tb in range(4):
                    tt = p * 4 + tb
                    banks = (0, 1) if tb % 2 == 0 else (2, 3)
                    for half in range(2):
                        bank = banks[half]

                        def mmT(e, tb=tb, half=half, bank=bank):
                            ins = None
                            for j in range(4):
                                dc = half * 4 + j
                                ins = e.matmul(psum[bank][:, j * 128:(j + 1) * 128], lhsT=x1b[:, dc, tb * 128:(tb + 1) * 128], rhs=identb, start=True, stop=True)
                            return ins

                        S.add("pe", mmT, reads=[gx1b, g_const], writes=[gps[bank]])
                        dstt = x1tok[:, tt, half * 512:(half + 1) * 512]
                        if half == 0:
                            S.add("act", lambda e, dstt=dstt, bank=bank: e.copy(out=dstt, in_=psum[bank]), reads=[gps[bank]], writes=[gx1tok[tt]])
                        else:
                            S.add("dve", lambda e, dstt=dstt, bank=bank: e.tensor_copy(out=dstt, in_=psum[bank]), reads=[gps[bank]], writes=[gx1tok[tt]])
                    os_ = tb % 2

                    def mmtr(e, tb=tb):
                        ins = None
                        for dc in range(NKC):
                            ins = e.transpose(psum[4 + dc // 4][:, (dc % 4) * 128:(dc % 4 + 1) * 128], accb[:, dc, tb * 128:(tb + 1) * 128], identf)
                        return ins

                    S.add("pe", mmtr, reads=gacc + [g_id], writes=[gps[4], gps[5]])
                    S.add("act", lambda e, os_=os_: e.copy(out=ostage[os_][:, 0:512], in_=psum[4]), reads=[gps[4]], writes=[gost[os_]])
                    S.add("dve", lambda e, os_=os_: e.tensor_copy(out=ostage[os_][:, 512:1024], in_=psum[5]), reads=[gps[5]], writes=[gost[os_]])
                    S.add("sp", lambda e, os_=os_, tb=tb: e.dma_start(out=x1f_d[t0 + tb * 128:t0 + (tb + 1) * 128, :], in_=ostage[os_]), reads=[gost[os_]], chan=cost[os_])
                    yield

        def phase_b(p):
            q = p % 2
            t0 = p * TP
            accb, gacc = accs[q], gaccs[q]
            if not last:
                def ln2_out(dc):
                    S.add("act", lambda e, dc=dc: e.activation(out=xo[q][:, dc, :], in_=accb[:, dc, :], func=AF.Identity, scale=pcol(lnb + 16 + dc), bias=pcol(lnb + 24 + dc)), reads=[gacc[dc], g_const], writes=[gxo[q]])

                yield from layer_norm(q, ln2_out)
                S.add("sp", lambda e: e.dma_start(out=xsB[:, :, t0:t0 + TP], in_=xo[q]), reads=[gxo[q]], chan=cxo[q])
                yield
            else:
                def ln2_out(dc):
                    S.add("act", lambda e, dc=dc: e.activation(out=accb[:, dc, :], in_=accb[:, dc, :], func=AF.Identity, scale=pcol(lnb + 16 + dc), bias=pcol(lnb + 24 + dc)), reads=[gacc[dc], g_const], writes=[gacc[dc]])

                yield from layer_norm(q, ln2_out)
                for tb in range(TP // 128):
                    os_ = tb % 2

                    def mmtr(e, tb=tb):
                        ins = None
                        for dc in range(NKC):
                            ins = e.transpose(psum[6 + dc // 4][:, (dc % 4) * 128:(dc % 4 + 1) * 128], accb[:, dc, tb * 128:(tb + 1) * 128], identf)
                        return ins

                    S.add("pe", mmtr, reads=gacc + [g_id], writes=[gps[6], gps[7]])
                    yield
                    S.add("act", lambda e, os_=os_: e.copy(out=ostage[os_][:, 0:512], in_=psum[6]), reads=[gps[6]], writes=[gost[os_]])
                    S.add("dve", lambda e, os_=os_: e.tensor_copy(out=ostage[os_][:, 512:1024], in_=psum[7]), reads=[gps[7]], writes=[gost[os_]])
                    S.add("sp", lambda e, os_=os_, tb=tb: e.dma_start(out=out_d[t0 + tb * 128:t0 + (tb + 1) * 128, :], in_=ostage[os_]), reads=[gost[os_]], chan=cost[os_])
                    yield

        I32 = mybir.dt.int32

        def moe_routed():
            for p in range(NP):
                for _ in phase_a(p):
                    pass
            if stop == "M_C1":
                return
            S.barrier()
            sb.off = arena1
            sh3 = [128, NTT, 8]
            mx = sb.alloc(sh3, F32, "mx")
            m1 = sb.alloc(sh3, F32, "m1")
            m12f = sb.alloc([128, NTT * 8], F32, "m12")
            m12 = m12f.rearrange("p (a b) -> p a b", b=8)
            m2 = sb.alloc(sh3, F32, "m2")
            tots = sb.alloc(sh3, F32, "tots")
            tbase = sb.alloc(sh3, F32, "tbase")
            pos = sb.alloc(sh3, F32, "pos")
            pt = sb.alloc(sh3, F32, "pt")
            dd = sb.alloc([128, NTT], F32, "dd")
            e2 = sb.alloc([128, NTT], F32, "e2")
            s1f = sb.alloc([128, NTT], F32, "s1f")
            s2f = sb.alloc([128, NTT], F32, "s2f")
            cnt = sb.alloc([128, 8], F32, "cnt")
            pad = sb.alloc([128, 8], F32, "pad")
            t8 = sb.alloc([128, 8], F32, "t8")
            ends = sb.alloc([128, 8], F32, "ends")
            starts = sb.alloc([128, 8], F32, "starts")
            ej = sb.alloc([128, NT], F32, "ej")
            basej = sb.alloc([128, NT], F32, "basej")
            widxf = sb.alloc([128, NT, NGE], F32, "widxf")
            gmx, gm1, gm12, gm2, gtots, gtbase, gpos, gpt, gdd, ge2, gs1f, gs2f, gcnt, gpad, gt8, gends, gstarts, gej, gbasej, gwidxf = (G() for _ in range(20))
            for tt in range(NTT):
                S.add("dve", lambda e, tt=tt: e.max(out=mx[:, tt, :], in_=lgall[:, tt, :]), reads=[glgall], writes=[gmx])
            S.add("dve", lambda e: e.tensor_tensor(out=m1, in0=lgall, in1=mx[:, :, 0:1].to_broadcast(sh3), op=ALU.is_ge), reads=[glgall, gmx], writes=[gm1])
            S.add("dve", lambda e: e.tensor_tensor(out=m12, in0=lgall, in1=mx[:, :, 1:2].to_broadcast(sh3), op=ALU.is_ge), reads=[glgall, gmx], writes=[gm12])
            S.add("dve", lambda e: e.tensor_tensor(out=m2, in0=m12, in1=m1, op=ALU.subtract), reads=[gm12, gm1], writes=[gm2])
            S.add("dve", lambda e: e.tensor_tensor(out=dd, in0=mx[:, :, 1], in1=mx[:, :, 0], op=ALU.subtract), reads=[gmx], writes=[gdd])
            S.add("act", lambda e: e.activation(out=e2, in_=dd, func=AF.Exp), reads=[gdd], writes=[ge2])
            S.add("dve", lambda e: e.tensor_scalar(out=dd, in0=e2, scalar1=1.0, scalar2=None, op0=ALU.add), reads=[ge2], writes=[gdd])
            S.add("dve", lambda e: e.reciprocal(out=w1, in_=dd), reads=[gdd], writes=[gw12])
            S.add("dve", lambda e: e.tensor_tensor(out=w2, in0=e2, in1=w1, op=ALU.mult), reads=[ge2, gw12], writes=[gw12])
            S.add("pe", lambda e: e.matmul(psum[0][:, 0:NTT * 8], lhsT=ltri, rhs=m12f, start=True, stop=True), reads=[gm12, g_const], writes=[gps[0]])
            S.add("pe", lambda e: e.matmul(psum[1][:, 0:NTT * 8], lhsT=onesf, rhs=m12f, start=True, stop=True), reads=[gm12, g_id], writes=[gps[1]])
            S.add("dve", lambda e: e.tensor_copy(out=tots, in_=psum[1][:, 0:NTT * 8].rearrange("p (a b) -> p a b", b=8)), reads=[gps[1]], writes=[gtots])
            S.add("pool", lambda e: e.memset(tbase[:, 0, :], 0.0), writes=[gtbase])
            for tt in range(1, NTT):
                S.add("dve", lambda e, tt=tt: e.tensor_tensor(out=tbase[:, tt, :], in0=tbase[:, tt - 1, :], in1=tots[:, tt - 1, :], op=ALU.add), reads=[gtots, gtbase], writes=[gtbase])
            S.add("dve", lambda e: e.tensor_tensor(out=cnt, in0=tbase[:, NTT - 1, :], in1=tots[:, NTT - 1, :], op=ALU.add), reads=[gtots, gtbase], writes=[gcnt])
            S.add("dve", lambda e: e.tensor_scalar(out=pad, in0=cnt, scalar1=0.0, scalar2=512.0, op0=ALU.is_gt, op1=ALU.mult), reads=[gcnt], writes=[gpad])
            for k in range(1, 8):
                S.add("dve", lambda e, k=k: e.tensor_scalar(out=t8, in0=cnt, scalar1=512.0 * k, scalar2=512.0, op0=ALU.is_gt, op1=ALU.mult), reads=[gcnt], writes=[gt8])
                S.add("dve", lambda e: e.tensor_tensor(out=pad, in0=pad, in1=t8, op=ALU.add), reads=[gt8, gpad], writes=[gpad])
            S.add("dve", lambda e: e.tensor_copy(out=ends[:, 0:1], in_=pad[:, 0:1]), reads=[gpad], writes=[gends])
            for ex in range(1, 8):
                S.add("dve", lambda e, ex=ex: e.tensor_tensor(out=ends[:, ex:ex + 1], in0=ends[:, ex - 1:ex], in1=pad[:, ex:ex + 1], op=ALU.add), reads=[gpad, gends], writes=[gends])
            S.add("dve", lambda e: e.tensor_tensor(out=starts, in0=ends, in1=pad, op=ALU.subtract), reads=[gpad, gends], writes=[gstarts])
            S.add("dve", lambda e: e.tensor_tensor(out=pos, in0=tbase, in1=psum[0][:, 0:NTT * 8].rearrange("p (a b) -> p a b", b=8), op=ALU.add), reads=[gps[0], gtbase], writes=[gpos])
            S.add("dve", lambda e: e.tensor_tensor(out=pos, in0=pos, in1=starts.unsqueeze(1).to_broadcast(sh3), op=ALU.add), reads=[gpos, gstarts], writes=[gpos])
            S.add("dve", lambda e: e.tensor_tensor(out=pt, in0=pos, in1=m1, op=ALU.mult), reads=[gpos, gm1], writes=[gpt])
            S.add("dve", lambda e: e.reduce_sum(out=s1f, in_=pt, axis=AX.X), reads=[gpt], writes=[gs1f])
            S.add("dve", lambda e: e.tensor_tensor(out=pt, in0=pos, in1=m2, op=ALU.mult), reads=[gpos, gm2, gs1f], writes=[gpt])
            S.add("dve", lambda e: e.reduce_sum(out=s2f, in_=pt, axis=AX.X), reads=[gpt], writes=[gs2f])
            S.add("dve", lambda e: e.tensor_copy(out=slot1i, in_=s1f), reads=[gs1f], writes=[gslot])
            S.add("dve", lambda e: e.tensor_copy(out=slot2i, in_=s2f), reads=[gs2f], writes=[gslot])
            for j in range(NT):
                S.add("dve", lambda e, j=j: e.tensor_scalar(out=t8, in0=ends, scalar1=512.0 * j, scalar2=None, op0=ALU.is_le), reads=[gends], writes=[gt8])
                S.add("dve", lambda e, j=j: e.reduce_sum(out=ej[:, j:j + 1], in_=t8, axis=AX.X), reads=[gt8], writes=[gej])
            S.add("dve", lambda e: e.tensor_scalar(out=ej, in0=ej, scalar1=float(NEXP - 1), scalar2=float(NGE * 128), op0=ALU.min, op1=ALU.mult), reads=[gej], writes=[gej])
            S.add("dve", lambda e: e.tensor_tensor(out=basej, in0=ej, in1=iotap.to_broadcast([128, NT]), op=ALU.add), reads=[gej, g_const], writes=[gbasej])
            for gi in range(NGE):
                S.add("dve", lambda e, gi=gi: e.tensor_scalar(out=widxf[:, :, gi], in0=basej, scalar1=128.0 * gi, scalar2=None, op0=ALU.add), reads=[gbasej], writes=[gwidxf])
            S.add("dve", lambda e: e.tensor_copy(out=widx, in_=widxf), reads=[gwidxf], writes=[gwidx])
            cscat = [S.chan(f"scat{i}") for i in range(2)]
            for tt in range(NTT):
                for k, sl in enumerate((slot1i, slot2i)):
                    S.add("pool", lambda e, tt=tt, sl=sl: e.indirect_dma_start(out=xg_d[:, :], out_offset=bass.IndirectOffsetOnAxis(ap=sl[:, tt:tt + 1], axis=0), in_=x1tok[:, tt, :], in_offset=None), reads=[gx1tok[tt], gslot], chan=cscat[k])
            S.barrier()
            if stop == "M_RT":
                return
            sb.off = xT_off
            NW = 3
            wgs = [sb.alloc([128, NKC * W], BF16, f"ewg{i}") for i in range(NW)]
            wus = [sb.alloc([128, NKC * W], BF16, f"ewu{i}") for i in range(NW)]
            wds = [sb.alloc([128, GC * DM], BF16, f"ewd{i}") for i in range(NW)]
            wgs3 = [t.rearrange("p (a b) -> p a b", a=NKC) for t in wgs]
            wus3 = [t.rearrange("p (a b) -> p a b", a=NKC) for t in wus]
            wds3 = [t.rearrange("p (a b) -> p a b", a=GC) for t in wds]
            gwgu = [G() for _ in range(NW)]
            gwd = [G() for _ in range(NW)]
            cgu = [S.chan(f"ewgu{i}") for i in range(NW)]
            cdd = [S.chan(f"ewdd{i}") for i in range(NW)]
            xgt = [sb.alloc([128, 4, DM], BF16, f"xgt{i}") for i in range(2)]
            gxgt = [G() for _ in range(2)]
            cxgt = [S.chan(f"xgt{i}") for i in range(2)]
            assert sb.off <= xT_off + NKC * SEQ * 2
            sb.off = arena1
            xe = [sb.alloc([128, NKC, 512], BF16, f"xe{i}") for i in range(2)]
            gxe = [G() for _ in range(2)]
            yacc = [sb.alloc([128, 4, DM], F32, f"yacc{i}") for i in range(2)]
            gy = [[G() for _ in range(8)] for _ in range(2)]
            cy = [S.chan(f"yst{i}") for i in range(2)]
            hT = [sb.alloc([128, GC, 512], BF16, f"ehT{i}") for i in range(2)]
            ghT = [G() for _ in range(2)]
            sg = [sb.alloc([128, 512], F32, f"esg{i}") for i in range(2)]
            gsg = [G() for _ in range(2)]
            NS = NT * NGE

            def prep(j):
                b = j % 2
                S.add("sp", lambda e: e.dma_start(out=xgt[b], in_=xg_d[j * 512:(j + 1) * 512, :].rearrange("(c p) d -> p c d", p=128)), writes=[gxgt[b]], chan=cxgt[b])
                for dc in range(NKC):
                    bank = 6 + dc % 2

                    def mm(e, dc=dc, bank=bank):
                        ins = None
                        for c in range(4):
                            ins = e.matmul(psum[bank][:, c * 128:(c + 1) * 128], lhsT=xgt[b][:, c, dc * 128:(dc + 1) * 128], rhs=identb, start=True, stop=True)
                        return ins

                    S.add("pe", mm, reads=[gxgt[b], g_const], writes=[gps[bank]])
                    if dc % 2 == 0:
                        S.add("act", lambda e, dc=dc, bank=bank: e.copy(out=xe[b][:, dc, :], in_=psum[bank]), reads=[gps[bank]], writes=[gxe[b]])
                    else:
                        S.add("dve", lambda e, dc=dc, bank=bank: e.tensor_copy(out=xe[b][:, dc, :], in_=psum[bank]), reads=[gps[bank]], writes=[gxe[b]])

            def load_w(i, part):
                j, gi = divmod(i, NGE)
                s = i % NW
                off = bass.IndirectOffsetOnAxis(ap=widx[:, j, gi:gi + 1], axis=0)
                if part == 0:
                    S.add("pool", lambda e: e.indirect_dma_start(out=wgs[s], out_offset=None, in_=wsc_gm[:, :], in_offset=off), reads=[gwidx], writes=[gwgu[s]], chan=cgu[s])
                    S.add("pool", lambda e: e.indirect_dma_start(out=wus[s], out_offset=None, in_=wsc_um[:, :], in_offset=off), reads=[gwidx], writes=[gwgu[s]], chan=cgu[s])
                else:
                    S.add("pool", lambda e: e.indirect_dma_start(out=wds[s], out_offset=None, in_=wsc_dm[:, :], in_offset=off), reads=[gwidx], writes=[gwd[s]], chan=cdd[s])

            def gate_up(i):
                j, gi = divmod(i, NGE)
                s = i % NW
                hs = i % 2
                b = j % 2
                for fc in range(GC):
                    k = fc % 2
                    bg_, bu_ = k, 2 + k

                    def mmg(e, fc=fc, bg_=bg_):
                        ins = None
                        for kc in range(NKC):
                            ins = e.matmul(psum[bg_], lhsT=wgs3[s][:, kc, fc * 128:(fc + 1) * 128], rhs=xe[b][:, kc, :], start=(kc == 0), stop=(kc == NKC - 1))
                        return ins

                    def mmu(e, fc=fc, bu_=bu_):
                        ins = None
                        for kc in range(NKC):
                            ins = e.matmul(psum[bu_], lhsT=wus3[s][:, kc, fc * 128:(fc + 1) * 128], rhs=xe[b][:, kc, :], start=(kc == 0), stop=(kc == NKC - 1))
                        return ins

                    S.add("pe", mmg, reads=[gwgu[s], gxe[b]], writes=[gps[bg_]])
                    S.add("pe", mmu, reads=[gwgu[s], gxe[b]], writes=[gps[bu_]])
                    S.add("act", lambda e, k=k, bg_=bg_: e.activation(out=sg[k], in_=psum[bg_], func=AF.Silu), reads=[gps[bg_]], writes=[gsg[k]])
                    S.add("dve", lambda e, fc=fc, k=k, bu_=bu_: e.tensor_tensor(out=hT[hs][:, fc, :], in0=sg[k], in1=psum[bu_], op=ALU.mult), reads=[gsg[k], gps[bu_]], writes=[ghT[hs]])

            def down(i):
                j, gi = divmod(i, NGE)
                s = i % NW
                hs = i % 2
                b = j % 2
                for c in range(4):
                    for half in range(2):
                        bd = 4 + half
                        gq_ = gy[b][c * 2 + half]

                        def mmd(e, c=c, half=half, bd=bd):
                            ins = None
                            for fc in range(GC):
                                ins = e.matmul(psum[bd], lhsT=hT[hs][:, fc, c * 128:(c + 1) * 128], rhs=wds3[s][:, fc, half * 512:(half + 1) * 512], start=(fc == 0), stop=(fc == GC - 1))
                            return ins

                        S.add("pe", mmd, reads=[gwd[s], ghT[hs]], writes=[gps[bd]])
                        dst = yacc[b][:, c, half * 512:(half + 1) * 512]
                        if gi == 0:
                            S.add("dve", lambda e, dst=dst, bd=bd: e.tensor_copy(out=dst, in_=psum[bd]), reads=[gps[bd]], writes=[gq_])
                        else:
                            S.add("dve", lambda e, dst=dst, bd=bd: e.tensor_tensor(out=dst, in0=dst, in1=psum[bd], op=ALU.add), reads=[gps[bd], gq_], writes=[gq_])
                if gi == NGE - 1:
                    S.add("sp", lambda e: e.dma_start(out=yd_d[j * 512:(j + 1) * 512, :].rearrange("(c p) d -> p c d", p=128), in_=yacc[b]), reads=gy[b], chan=cy[b])

            prep(0)
            for i in range(min(NW - 1, NS)):
                load_w(i, 0)
                load_w(i, 1)
            for i in range(NS):
                j, gi = divmod(i, NGE)
                if i + NW - 1 < NS:
                    load_w(i + NW - 1, 0)
                if gi == 6 and j + 1 < NT:
                    prep(j + 1)
                gate_up(i)
                if i > 0:
                    down(i - 1)
                if i + NW - 1 < NS:
                    load_w(i + NW - 1, 1)
            down(NS - 1)
            S.barrier()
            if stop == "M_EX":
                return
            sb.off = xT_off
            gb = sb.alloc([128, 2, DM], F32, "gb")
            ggb = G()
            cgb = S.chan("gb")
            S.add("sp", lambda e: e.dma_start(out=gb, in_=ln2row_d), writes=[ggb], chan=cgb)
            NBC = 4
            sb.off = arena1
            y1 = [sb.alloc([128, DM], F32, f"y1_{i}") for i in range(NBC)]
            y2 = [sb.alloc([128, DM], F32, f"y2_{i}") for i in range(NBC)]
            xr = [sb.alloc([128, DM], F32, f"xr{i}") for i in range(NBC)]
            zq = [sb.alloc([128, DM], F32, f"zq{i}") for i in range(NBC)]
            stc = [sb.alloc([128, 8], F32, f"stc{i}") for i in range(NBC)]
            gy1 = [G() for _ in range(NBC)]
            gy2 = [G() for _ in range(NBC)]
            gxr = [G() for _ in range(NBC)]
            gzq = [G() for _ in range(NBC)]
            gstc = [G() for _ in range(NBC)]
            cg1 = [S.chan(f"cg1_{i}") for i in range(NBC)]
            cg2 = [S.chan(f"cg2_{i}") for i in range(NBC)]
            cxr = [S.chan(f"cxr{i}") for i in range(NBC)]
            cfo = [S.chan(f"cfo{i}") for i in range(NBC)]
            for tt in range(NTT):
                b = tt % NBC
                st = stc[b]
                S.add("pool", lambda e, tt=tt, b=b: e.indirect_dma_start(out=y1[b], out_offset=None, in_=yd_d[:, :], in_offset=bass.IndirectOffsetOnAxis(ap=slot1i[:, tt:tt + 1], axis=0)), reads=[gslot], writes=[gy1[b]], chan=cg1[b])
                S.add("pool", lambda e, tt=tt, b=b: e.indirect_dma_start(out=y2[b], out_offset=None, in_=yd_d[:, :], in_offset=bass.IndirectOffsetOnAxis(ap=slot2i[:, tt:tt + 1], axis=0)), reads=[gslot], writes=[gy2[b]], chan=cg2[b])
                S.add("sp", lambda e, tt=tt, b=b: e.dma_start(out=xr[b], in_=x1f_d[tt * 128:(tt + 1) * 128, :]), writes=[gxr[b]], chan=cxr[b])
                S.add("dve", lambda e, tt=tt, b=b: e.scalar_tensor_tensor(out=xr[b], in0=y1[b], scalar=w1[:, tt:tt + 1], in1=xr[b], op0=ALU.mult, op1=ALU.add), reads=[gy1[b], gxr[b], gw12], writes=[gxr[b]])
                S.add("dve", lambda e, tt=tt, b=b: e.scalar_tensor_tensor(out=xr[b], in0=y2[b], scalar=w2[:, tt:tt + 1], in1=xr[b], op0=ALU.mult, op1=ALU.add), reads=[gy2[b], gxr[b], gw12], writes=[gxr[b]])
                S.add("act", lambda e, b=b: e.activation(out=zq[b], in_=xr[b], func=AF.Square), reads=[gxr[b]], writes=[gzq[b]])
                S.add("dve", lambda e, b=b, st=st: e.reduce_sum(out=st[:, 0:1], in_=xr[b], axis=AX.X), reads=[gxr[b]], writes=[gstc[b]])
                S.add("dve", lambda e, b=b, st=st: e.reduce_sum(out=st[:, 1:2], in_=zq[b], axis=AX.X), reads=[gzq[b]], writes=[gstc[b]])
                S.add("dve", lambda e, st=st: e.tensor_scalar(out=st[:, 2:3], in0=st[:, 0:1], scalar1=1.0 / DM, scalar2=None, op0=ALU.mult), reads=[gstc[b]], writes=[gstc[b]])
                S.add("dve", lambda e, st=st: e.tensor_tensor(out=st[:, 3:4], in0=st[:, 2:3], in1=st[:, 2:3], op=ALU.mult), reads=[gstc[b]], writes=[gstc[b]])
                S.add("dve", lambda e, st=st: e.scalar_tensor_tensor(out=st[:, 4:5], in0=st[:, 1:2], scalar=1.0 / DM, in1=st[:, 3:4], op0=ALU.mult, op1=ALU.subtract), reads=[gstc[b]], writes=[gstc[b]])
                S.add("dve", lambda e, st=st: e.tensor_scalar(out=st[:, 4:5], in0=st[:, 4:5], scalar1=LN_EPS, scalar2=None, op0=ALU.add), reads=[gstc[b]], writes=[gstc[b]])
                S.add("act", lambda e, st=st: e.activation(out=st[:, 4:5], in_=st[:, 4:5], func=AF.Sqrt), reads=[gstc[b]], writes=[gstc[b]])
                S.add("dve", lambda e, st=st: e.reciprocal(out=st[:, 5:6], in_=st[:, 4:5]), reads=[gstc[b]], writes=[gstc[b]])
                S.add("dve", lambda e, b=b, st=st: e.scalar_tensor_tensor(out=zq[b], in0=xr[b], scalar=st[:, 2:3], in1=gb[:, 0, :], op0=ALU.subtract, op1=ALU.mult), reads=[gxr[b], gstc[b], gzq[b], ggb], writes=[gzq[b]])
                S.add("dve", lambda e, b=b, st=st: e.scalar_tensor_tensor(out=zq[b], in0=zq[b], scalar=st[:, 5:6], in1=gb[:, 1, :], op0=ALU.mult, op1=ALU.add), reads=[gzq[b], gstc[b], ggb], writes=[gzq[b]])
                S.add("sp", lambda e, tt=tt, b=b: e.dma_start(out=out_d[tt * 128:(tt + 1) * 128, :], in_=zq[b]), reads=[gzq[b]], chan=cfo[b])

        nexp = NEXP if moe else 1
        ng = (EFF if moe else DFF) // W
        stages = [(ex, gi) for ex in range(nexp) for gi in range(ng)]

        def ffn(p, inter, per_stage):
            q = p % 2
            accb, gacc, x1b, gx1b = accs[q], gaccs[q], x1bs[q], gx1bs[q]
            xup = x1c if moe else x1b
            gxup = gx1c if moe else gx1b

            def load_w(i, part):
                ex, gi = stages[i]
                s = i % 2
                if p == 0:
                    if moe:
                        srcs = (mg_d[ex, gi], mu_d[ex, gi], md_d[ex, gi])
                    else:
                        srcs = (fg_d[gi], fu_d[gi], fd_d[gi])
                    if part == 0:
                        S.add("pool", lambda e, s=s, src=srcs[0]: e.dma_start(out=wgs[s], in_=src), writes=[gwgu[s]], chan=cgu[s])
                        S.add("pool", lambda e, s=s, src=srcs[1]: e.dma_start(out=wus[s], in_=src), writes=[gwgu[s]], chan=cgu[s])
                        S.add("sp", lambda e, s=s, i=i: e.dma_start(out=wsc_g[i], in_=wgs[s]), reads=[gwgu[s]], writes=[gscr[i]], chan=cwb[s])
                        S.add("sp", lambda e, s=s, i=i: e.dma_start(out=wsc_u[i], in_=wus[s]), reads=[gwgu[s]], writes=[gscr[i]], chan=cwb[s])
                    else:
                        S.add("pool", lambda e, s=s, src=srcs[2]: e.dma_start(out=wds[s], in_=src), writes=[gwd[s]], chan=cdd[s])
                        S.add("sp", lambda e, s=s, i=i: e.dma_start(out=wsc_d[i], in_=wds[s]), reads=[gwd[s]], writes=[gscrd[i]], chan=cwbd[s])
                else:
                    if part == 0:
                        S.add("sp", lambda e, s=s, i=i: e.dma_start(out=wgs[s], in_=wsc_g[i]), reads=[gscr[i]], writes=[gwgu[s]], chan=cguh[s])
                        S.add("sp", lambda e, s=s, i=i: e.dma_start(out=wus[s], in_=wsc_u[i]), reads=[gscr[i]], writes=[gwgu[s]], chan=cguh[s])
                    else:
                        S.add("sp", lambda e, s=s, i=i: e.dma_start(out=wds[s], in_=wsc_d[i]), reads=[gscrd[i]], writes=[gwd[s]], chan=cddh[s])

            def gate_up(i):
                s = i % 2
                for fc in range(GC):
                    k = fc % 2
                    bg_, bu_ = k, 2 + k

                    def mmg(e, s=s, fc=fc, bg_=bg_):
                        ins = None
                        for kc in range(NKC):
                            ins = e.matmul(psum[bg_], lhsT=wgs[s][:, kc, fc * 128:(fc + 1) * 128], rhs=x1b[:, kc, :], start=(kc == 0), stop=(kc == NKC - 1))
                        return ins

                    def mmu(e, s=s, fc=fc, bu_=bu_):
                        ins = None
                        for kc in range(NKC):
                            ins = e.matmul(psum[bu_], lhsT=wus[s][:, kc, fc * 128:(fc + 1) * 128], rhs=xup[:, kc, :], start=(kc == 0), stop=(kc == NKC - 1))
                        return ins

                    S.add("pe", mmg, reads=[gwgu[s], gx1b], writes=[gps[bg_]])
                    S.add("pe", mmu, reads=[gwgu[s], gxup], writes=[gps[bu_]])
                    S.add("act", lambda e, k=k, bg_=bg_: e.activation(out=sg[k], in_=psum[bg_], func=AF.Silu), reads=[gps[bg_]], writes=[gsg[k]])
                    S.add("dve", lambda e, s=s, fc=fc, k=k, bu_=bu_: e.tensor_tensor(out=hT[s][:, fc, :], in0=sg[k], in1=psum[bu_], op=ALU.mult), reads=[gsg[k], gps[bu_]], writes=[ghT[s]])
                    pull(1)

            def down(i):
                s = i % 2
                for dc in range(NKC):
                    bd = 4 + dc % 2

                    def mmd(e, s=s, dc=dc, bd=bd):
                        ins = None
                        for fc in range(GC):
                            ins = e.matmul(psum[bd], lhsT=wds[s][:, fc, dc * 128:(dc + 1) * 128], rhs=hT[s][:, fc, :], start=(fc == 0), stop=(fc == GC - 1))
                        return ins

                    S.add("pe", mmd, reads=[gwd[s], ghT[s]], writes=[gps[bd]])
                    S.add("dve", lambda e, dc=dc, bd=bd: e.tensor_tensor(out=accb[:, dc, :], in0=accb[:, dc, :], in1=psum[bd], op=ALU.add), reads=[gps[bd], gacc[dc]], writes=[gacc[dc]])

            def pull(n):
                for _ in range(n):
                    if next(inter, "done") == "done":
                        return

            load_w(0, 0)
            load_w(0, 1)
            for i, (ex, gi) in enumerate(stages):
                if i + 1 < len(stages):
                    load_w(i + 1, 0)
                if moe and gi == 0:
                    S.add("pe", lambda e, ex=ex: e.matmul(psum[5], lhsT=selb[:, ex, :], rhs=combTs[q], start=True, stop=True), reads=[gcombTs[q], g_const], writes=[gps[5]])
                    for kc in range(NKC):
                        S.add("dve", lambda e, kc=kc: e.tensor_tensor(out=x1c[:, kc, :], in0=x1b[:, kc, :], in1=psum[5], op=ALU.mult), reads=[gps[5], gx1b], writes=[gx1c])
                gate_up(i)
                if i > 0:
                    down(i - 1)
                if i + 1 < len(stages):
                    load_w(i + 1, 1)
                pull(max(per_stage - GC, 0))
            down(len(stages) - 1)
            pull(10 ** 6)

        def chain_(*gens):
            for g_ in gens:
                yield from g_

        if moe:
            moe_routed()
            return False
        for _ in phase_a(0):
            pass
        per_stage = 1 if moe else 3
        for p in range(NP):
            gens = []
            if p > 0:
                gens.append(phase_b(p - 1))
            if p + 1 < NP:
                gens.append(phase_a(p + 1))
            ffn(p, chain_(*gens), per_stage)
        for _ in phase_b(NP - 1):
            pass
        if not last:
            S.barrier()
            crl = S.chan("reload")
            for t in range(8):
                S.add("sp", lambda e, t=t: e.dma_start(out=xT[:, :, t * 512:(t + 1) * 512], in_=xsB[:, :, t * 512:(t + 1) * 512]), writes=[gxT[t]], chan=crl)
        if stop == f"C{layer}":
            return True
        return False

    for layer_i in range(NL):
        if layer_body(layer_i):
            break
    S.barrier()
    if "xT" in dbg:
        dump_xT()
    if "y" in dbg:
        sb.off = arena0
        tb = sb.alloc([128, SEQ], BF16, "dbgy")
        tf = sb.alloc([128, SEQ], F32, "dbgyf")
        g1, g2 = G(), G()
        cd = S.chan("dbgy")
        for c in range(8):
            S.add("sp", lambda e, c=c: e.dma_start(out=tb, in_=yscr[c]), writes=[g1], chan=cd)
            S.add("dve", lambda e: e.tensor_copy(out=tf, in_=tb), reads=[g1], writes=[g2])
            S.add("sp", lambda e, c=c: e.dma_start(out=dbg["y"][c], in_=tf), reads=[g2], chan=c_out)
    S.emit(nc)
    return nc


_CACHE = {}


def kernel(**inputs):
    sh = _prep_shared(inputs)
    x = np.ascontiguousarray(inputs["x"], dtype=np.float32)
    if "nc" not in _CACHE:
        _CACHE["nc"] = build_program()
    nc = _CACHE["nc"]
    in_maps = []
    for b in range(x.shape[0]):
        m = dict(sh)
        m["x"] = np.ascontiguousarray(x[b])
        in_maps.append(m)
    res = run_bass_kernel_spmd(nc, in_maps, core_ids=list(range(x.shape[0])))
    return np.stack([np.asarray(r["out"]) for r in res.results], axis=0).astype(np.float32)
```

```python
import numpy as np
import concourse.bass as bass
import concourse.mybir as mybir
from concourse.bass_utils import run_bass_kernel_spmd

F32 = mybir.dt.float32
BF16 = mybir.dt.bfloat16
ALU = mybir.AluOpType
AF = mybir.ActivationFunctionType
AX = mybir.AxisListType

NL = 2
SEQ = 4096
DM = 1024
NKC = 8
ALPHA = (2.0 * NL) ** 0.25
LN_EPS = 1e-5
RMS_EPS = 1e-6
PATTERNS = ((128, 1), (512, 4), (2048, 16))
NEG = -30000.0
DFF = 2816
EFF = 3584
NEXP = 8
GC = 2
TP = 512
ST = 512


class G:
    __slots__ = ("w", "r", "name")

    def __init__(self, name=""):
        self.w = None
        self.r = {}
        self.name = name


class Chan:
    def __init__(self, name):
        self.name = name
        self.count = 0
        self.sem = None
        self.last = None


class Op:
    __slots__ = ("eng", "fn", "order", "waits", "sig", "chan", "cnt", "epoch")


class Sched:
    ENGS = ("pe", "act", "dve", "pool", "sp")
    EPOCH_LIMIT = 20000

    def __init__(self):
        self.ops = {e: [] for e in self.ENGS}
        self.chans = []

    def chan(self, name):
        c = Chan(name)
        self.chans.append(c)
        return c

    def add(self, eng, fn, reads=(), writes=(), chan=None):
        op = Op()
        op.eng = eng
        op.fn = fn
        op.chan = chan
        op.sig = False
        op.cnt = None
        op.epoch = None
        op.order = len(self.ops[eng])
        if chan is not None:
            chan.count += 1
            op.cnt = chan.count
            chan.last = op
        need = {}

        def consider(d, raw):
            if d is op:
                return
            if d.chan is None and d.eng == eng:
                if (not raw) or eng == "pe":
                    return
            key = ("c", id(d.chan)) if d.chan is not None else ("e", d.eng)
            prev = need.get(key)
            if prev is None:
                need[key] = d
            else:
                a = d.cnt if d.chan is not None else d.order
                b = prev.cnt if prev.chan is not None else prev.order
                if a > b:
                    need[key] = d

        for t in reads:
            if t.w is not None:
                consider(t.w, True)
        for t in writes:
            if t.w is not None:
                consider(t.w, False)
            for r in t.r.values():
                consider(r, False)
        op.waits = list(need.values())
        for d in op.waits:
            d.sig = True
        for t in writes:
            t.w = op
            t.r = {}
        rk = ("c", id(chan)) if chan is not None else ("e", eng)
        for t in reads:
            if t.w is not op:
                t.r[rk] = op
        self.ops[eng].append(op)
        return op

    def barrier(self):
        gs = []
        for e in self.ENGS:
            real = [o for o in self.ops[e][-64:] if o.fn is not None] or [o for o in self.ops[e] if o.fn is not None]
            if real:
                g = G("bar")
                g.w = real[-1]
                gs.append(g)
        for c in self.chans:
            if c.last is not None:
                g = G("barc")
                g.w = c.last
                gs.append(g)
        for e in self.ENGS:
            self.add(e, None, reads=gs)

    def emit(self, nc, final_eng="sp"):
        nep = {}
        for e in self.ENGS:
            ep = 0
            c = 0
            for op in self.ops[e]:
                if op.chan is None and op.sig:
                    if c >= self.EPOCH_LIMIT:
                        ep += 1
                        c = 0
                    c += 1
                    op.cnt = c
                    op.epoch = ep
            nep[e] = ep + 1
        sems = {}
        for e in self.ENGS:
            for ep in range(nep[e]):
                sems[(e, ep)] = nc.alloc_semaphore(f"s_{e}_{ep}")
        for i, c in enumerate(self.chans):
            c.sem = nc.alloc_semaphore(f"c_{i}_{c.name}")
        self.nsem = len(sems) + len(self.chans)
        engobj = {"pe": "tensor", "act": "scalar", "dve": "vector", "pool": "gpsimd", "sp": "sync"}

        def run_engine(e, eng):
            seen = {}
            for op in self.ops[e]:
                for d in op.waits:
                    if d.chan is not None:
                        sem, val = d.chan.sem, 16 * d.cnt
                    else:
                        sem, val = sems[(d.eng, d.epoch)], d.cnt
                    k = id(sem)
                    if seen.get(k, 0) >= val:
                        continue
                    seen[k] = val
                    eng.wait_ge(sem, val)
                if op.fn is None:
                    continue
                ins = op.fn(eng)
                if op.chan is not None:
                    ins.then_inc(op.chan.sem, 16)
                elif op.sig:
                    ins.then_inc(sems[(e, op.epoch)], 1)
            if e == final_eng:
                for c in self.chans:
                    if c.count > 0:
                        k = id(c.sem)
                        if seen.get(k, 0) < 16 * c.count:
                            eng.wait_ge(c.sem, 16 * c.count)

        with nc.Block() as block:
            for e in self.ENGS:
                deco = getattr(block, engobj[e])

                def mk(e):
                    def f(eng):
                        run_engine(e, eng)

                    return f

                deco(mk(e))


def _t5_bucket(dist):
    n_buckets, max_distance = 32, 2048
    max_exact = n_buckets // 2
    d = np.maximum(dist, 1).astype(np.float32)
    large = max_exact + (np.log(d / max_exact) / np.log(max_distance / max_exact) * (n_buckets - max_exact)).astype(np.int32)
    large = np.minimum(large, n_buckets - 1)
    return np.where(dist < max_exact, dist, large).astype(np.int32)


PP_LRU = 0
PP_GMIX = 64
PP_LN = 80
NPP = 80 + 64


def _prep_shared(inp):
    f = np.float32
    sh = {}
    w_in = inp["w_in"].astype(f, copy=False)
    sh["w_in_r"] = np.ascontiguousarray(w_in.reshape(NL, NKC, 128, 20, 128).transpose(0, 3, 2, 1, 4))
    w_out = inp["w_out"].astype(f, copy=False)
    sh["w_out_r"] = np.ascontiguousarray(w_out.reshape(NL, NKC, 128, DM).transpose(0, 2, 1, 3))
    wg = np.zeros((NL, 2, 4, 128, 128), f)
    for which, name in enumerate(("w_a", "w_x")):
        w = inp[name]
        for c in range(4):
            wg[:, which, c, 0:64, 0:64] = w[:, 2 * c]
            wg[:, which, c, 64:128, 64:128] = w[:, 2 * c + 1]
    sh["wgate_r"] = np.ascontiguousarray(wg.transpose(3, 0, 1, 2, 4).reshape(128, 16, 128))
    pp = np.zeros((128, NPP), f)
    for l in range(NL):
        for c in range(4):
            base = PP_LRU + (l * 4 + c) * 8
            sl = slice(c * 128, (c + 1) * 128)
            for j in range(4):
                pp[:, base + j] = inp["conv_w"][l, j, 0, sl]
            pp[:, base + 4] = inp["conv_b"][l, sl]
            pp[:, base + 5] = inp["b_a"][l, sl]
            pp[:, base + 6] = inp["b_x"][l, sl]
            pp[:, base + 7] = inp["lru_lambda"][l, sl]
        for c in range(4):
            pp[:, PP_GMIX + l * 8 + c] = inp["g_attn"][l, c * 128:(c + 1) * 128]
            pp[:, PP_GMIX + l * 8 + 4 + c] = inp["g_lru"][l, c * 128:(c + 1) * 128]
        for k, name in enumerate(("ln1_g", "ln1_b", "ln2_g", "ln2_b")):
            for c in range(8):
                pp[:, PP_LN + (l * 4 + k) * 8 + c] = inp[name][l, c * 128:(c + 1) * 128]
    sh["pp"] = pp
    rel = inp["rel_bias"].astype(f, copy=False)
    tab = np.full((128, 3, 8, 256), NEG, f)
    kk = np.arange(128)[:, None]
    qq = np.arange(128)[None, :]
    for pi, (window, dil) in enumerate(PATTERNS):
        dc = qq - kk
        bc = _t5_bucket(np.clip(dc, 0, 128) * dil)
        dp = qq + 128 - kk
        bp = _t5_bucket(np.clip(dp, 0, 128) * dil)
        for h in range(8):
            cur = np.where(dc >= 0, rel[bc, h], f(NEG))
            prv = np.where(dp <= 128, rel[bp, h], f(NEG))
            tab[:, pi, h, 0:128] = cur
            tab[:, pi, h, 128:256] = prv
    sh["tab"] = tab
    W = GC * 128
    ng = DFF // W
    sh["fg_r"] = np.ascontiguousarray(inp["ffn_w_gate"][0].reshape(NKC, 128, ng, W).transpose(2, 1, 0, 3))
    sh["fu_r"] = np.ascontiguousarray(inp["ffn_w_up"][0].reshape(NKC, 128, ng, W).transpose(2, 1, 0, 3))
    sh["fd_r"] = np.ascontiguousarray(inp["ffn_w_down"][0].reshape(ng, GC, 128, DM).transpose(0, 2, 1, 3))
    ng = EFF // W
    sh["mg_r"] = np.ascontiguousarray(inp["moe_w_gate"][0].reshape(NEXP, NKC, 128, ng, W).transpose(0, 3, 2, 1, 4))
    sh["mu_r"] = np.ascontiguousarray(inp["moe_w_up"][0].reshape(NEXP, NKC, 128, ng, W).transpose(0, 3, 2, 1, 4))
    sh["md_r"] = np.ascontiguousarray(inp["moe_w_down"][0].reshape(NEXP, ng, GC, 128, DM).transpose(0, 1, 3, 2, 4))
    sh["router_r"] = np.ascontiguousarray(inp["router_w"][0].reshape(NKC, 128, NEXP).transpose(1, 0, 2))
    sel = np.zeros((8, 8, 128), f)
    for e in range(8):
        sel[e, e, :] = 1.0
    sh["sel"] = sel
    sh["ltri"] = np.triu(np.ones((128, 128), f), 1)
    sh["iotap"] = np.arange(128, dtype=f).reshape(128, 1)
    l2 = np.stack([inp["ln2_g"][NL - 1], inp["ln2_b"][NL - 1]], 0).astype(f)
    sh["ln2row"] = np.ascontiguousarray(np.broadcast_to(l2[None], (128, 2, DM)))
    return sh


class SB:
    def __init__(self, nc):
        self.nc = nc
        total = int(nc.SBUF_PARTITION_SIZE_BYTES)
        self.off = (total - int(nc.sbuf_bytes_remaining) + 255) // 256 * 256
        self.n = 0
        self.limit = 208 * 1024

    regions = None
    ridx = 0

    def alloc(self, shape, dtype, name=None):
        nbytes = int(np.prod(shape[1:])) * (2 if dtype == BF16 else 4)
        self.off = (self.off + 63) // 64 * 64
        if self.regions is not None:
            while self.off + nbytes > self.regions[self.ridx][1]:
                self.ridx += 1
                assert self.ridx < len(self.regions), (name, "SBUF regions exhausted")
                self.off = (self.regions[self.ridx][0] + 63) // 64 * 64
        self.n += 1
        t = self.nc.alloc_sbuf_tensor_at(f"{name or 'b'}_{self.n}", list(shape), dtype, offset=self.off)
        self.off += nbytes
        assert self.off <= self.limit, (name, self.off)
        return t.ap()


def build_program(debug=None, stop=None):
    debug = debug or ()
    nc = bass.Bass("TRN2", target_bir_lowering=False)
    S = Sched()

    def din(name, shape, dt=F32):
        return nc.dram_tensor(name, list(shape), dt, kind="ExternalInput").ap()

    W = GC * 128
    x_d = din("x", [SEQ, DM])
    w_in_d = din("w_in_r", [NL, 20, 128, NKC, 128])
    w_out_d = din("w_out_r", [NL, 128, NKC, DM])
    wgate_d = din("wgate_r", [128, 16, 128])
    pp_d = din("pp", [128, NPP])
    tab_d = din("tab", [128, 3, 8, 256])
    fg_d = din("fg_r", [DFF // W, 128, NKC, W])
    fu_d = din("fu_r", [DFF // W, 128, NKC, W])
    fd_d = din("fd_r", [DFF // W, 128, GC, DM])
    mg_d = din("mg_r", [NEXP, EFF // W, 128, NKC, W])
    mu_d = din("mu_r", [NEXP, EFF // W, 128, NKC, W])
    md_d = din("md_r", [NEXP, EFF // W, 128, GC, DM])
    router_d = din("router_r", [128, NKC, NEXP])
    sel_d = din("sel", [8, 8, 128])
    ltri_d = din("ltri", [128, 128])
    iotap_d = din("iotap", [128, 1])
    ln2row_d = din("ln2row", [128, 2, DM])
    NGE = EFF // W
    NST = NEXP * NGE
    NT = 23
    wsc_gm = nc.dram_tensor("wsc_gm", [NST * 128, NKC * W], BF16).ap()
    wsc_um = nc.dram_tensor("wsc_um", [NST * 128, NKC * W], BF16).ap()
    wsc_dm = nc.dram_tensor("wsc_dm", [NST * 128, GC * DM], BF16).ap()
    xg_d = nc.dram_tensor("xg", [NT * 512, DM], BF16).ap()
    yd_d = nc.dram_tensor("yd", [NT * 512, DM], F32).ap()
    x1f_d = nc.dram_tensor("x1f", [SEQ, DM], F32).ap()
    out_d = nc.dram_tensor("out", [SEQ, DM], F32, kind="ExternalOutput").ap()
    yscr = nc.dram_tensor("yscr", [8, 128, SEQ], BF16).ap()
    xsA = nc.dram_tensor("xsA", [128, NKC, SEQ], BF16).ap()
    xsB = nc.dram_tensor("xsB", [128, NKC, SEQ], BF16).ap()
    dbg = {}
    for name, shape in (("xT", [128, NKC, SEQ]), ("y", [8, 128, SEQ])):
        if ("dbg_" + name) in debug:
            dbg[name] = nc.dram_tensor("dbg_" + name, shape, F32, kind="ExternalOutput").ap()

    dumped = set()

    def dump(name, ap, reads):
        key = "dbg_" + name
        if key not in debug or key in dumped:
            return
        dumped.add(key)
        dt_ = nc.dram_tensor(key, list(ap.shape), ap.dtype, kind="ExternalOutput").ap()
        S.add("sp", lambda e: e.dma_start(out=dt_, in_=ap), reads=reads, chan=c_out)

    sb = SB(nc)
    xT_off = sb.off
    xT = sb.alloc([128, NKC, SEQ], BF16, "xT")
    gxT = [G(f"xT{i}") for i in range(SEQ // 512)]
    identf = sb.alloc([128, 128], F32, "identf")
    identb = sb.alloc([128, 128], BF16, "identb")
    onesb = sb.alloc([128, 128], BF16, "onesb")
    pp = sb.alloc([128, NPP], F32, "pp")
    ppx = sb.alloc([128, 64], F32, "ppx")
    wgate = sb.alloc([128, 16, 128], BF16, "wgate")
    routerb = sb.alloc([128, NKC, NEXP], F32, "routerb")
    g_const = G("const")
    g_ppx = G("ppx")
    tmp8 = sb.alloc([128, 8], F32, "tmp8")
    tmp8b = sb.alloc([128, 8], F32, "tmp8b")
    ltri = sb.alloc([128, 128], F32, "ltri")
    onesf = sb.alloc([128, 128], F32, "onesf")
    iotap = sb.alloc([128, 1], F32, "iotap")
    cb_off = sb.off
    NCB = 3
    cbuf = [sb.alloc([128, NKC * W], BF16, f"cbuf{i}") for i in range(NCB)]
    gcb = [G() for _ in range(NCB)]
    arena0 = sb.off

    psum = [nc.alloc_psum_tensor(f"ps{i}", [128, 512], F32).ap() for i in range(8)]
    gps = [G(f"ps{i}") for i in range(8)]

    c_const = S.chan("const")
    c_out = S.chan("out")

    def xg(t0, t1):
        return gxT[t0 // 512:(t1 + 511) // 512]

    S.add("sp", lambda e: e.dma_start(out=pp, in_=pp_d), writes=[g_const], chan=c_const)
    S.add("sp", lambda e: e.dma_start(out=routerb, in_=router_d), writes=[g_const], chan=c_const)
    c_const_sw = S.chan("const_sw")
    S.add("pool", lambda e: e.dma_start(out=wgate, in_=wgate_d), writes=[g_const], chan=c_const_sw)
    S.add("sp", lambda e: e.dma_start(out=ltri, in_=ltri_d), writes=[g_const], chan=c_const)
    S.add("sp", lambda e: e.dma_start(out=iotap, in_=iotap_d), writes=[g_const], chan=c_const)
    g_id = G("ident")
    ccl = [S.chan(f"cvl{i}") for i in range(NCB)]
    ccs = [S.chan(f"cvs{i}") for i in range(NCB)]

    NSD = DFF // W
    wsc_g0 = nc.dram_tensor("wsc_g0", [NSD, 128, NKC, W], BF16).ap()
    wsc_u0 = nc.dram_tensor("wsc_u0", [NSD, 128, NKC, W], BF16).ap()
    wsc_d0 = nc.dram_tensor("wsc_d0", [NSD, 128, GC, DM], BF16).ap()

    def conv_gen():
        k = 0
        for ex in range(NEXP):
            for gi in range(NGE):
                i = ex * NGE + gi
                for src, dst, a in ((mg_d[ex, gi], wsc_gm, NKC), (mu_d[ex, gi], wsc_um, NKC), (md_d[ex, gi], wsc_dm, GC)):
                    b = k % NCB
                    k += 1
                    S.add("pool", lambda e, b=b, src=src, a=a: e.dma_start(out=cbuf[b].rearrange("p (a b) -> p a b", a=a), in_=src), writes=[gcb[b]], chan=ccl[b])
                    S.add("sp", lambda e, b=b, dst=dst, i=i: e.dma_start(out=dst[i * 128:(i + 1) * 128, :], in_=cbuf[b]), reads=[gcb[b]], chan=ccs[b])
                    yield

    conv = conv_gen()

    def conv_pull(n):
        for _ in range(n):
            if next(conv, "done") == "done":
                return

    g_idm = G("identm")

    def mk_ones(e):
        e.memset(onesf, 1.0)
        return e.memset(onesb, 1.0)

    S.add("pool", mk_ones, writes=[g_id])
    S.add("pool", lambda e: e.memset(identf, 0.0), writes=[g_idm])
    S.add("pool", lambda e: e.affine_select(out=identf, in_=identf, pattern=[[-1, 128]], compare_op=ALU.not_equal, fill=1.0, base=0, channel_multiplier=1), reads=[g_idm], writes=[g_id])
    S.add("dve", lambda e: e.tensor_copy(out=identb, in_=identf), reads=[g_id], writes=[g_const])
    g_t1 = G("t1")
    lam = pp.rearrange("p (a b) -> p a b", b=8)[:, 0:8, 7]
    S.add("act", lambda e: e.activation(out=tmp8, in_=lam, func=AF.Exp, scale=-1.0), reads=[g_const], writes=[g_t1])
    g_t2 = G("t2")
    S.add("act", lambda e: e.activation(out=tmp8b, in_=tmp8, func=AF.Ln, bias=1.0, scale=1.0), reads=[g_t1], writes=[g_t2])
    S.add("dve", lambda e: e.tensor_scalar(out=ppx[:, 0:8], in0=tmp8b, scalar1=-8.0, scalar2=None, op0=ALU.mult), reads=[g_t2], writes=[g_ppx])
    S.add("dve", lambda e: e.tensor_scalar(out=ppx[:, 16:16 + 32], in0=pp[:, PP_LN:PP_LN + 32], scalar1=ALPHA, scalar2=None, op0=ALU.mult), reads=[g_const], writes=[g_ppx])
    S.add("dve", lambda e: e.tensor_scalar(out=ppx[:, 32:48], in0=pp[:, PP_LN + 32:PP_LN + 48], scalar1=ALPHA, scalar2=None, op0=ALU.mult), reads=[g_const], writes=[g_ppx])

    def pcol(i):
        return pp[:, i:i + 1]

    sb.off = arena0
    xs = [sb.alloc([128, DM], F32, f"xs{i}") for i in range(2)]
    gxs = [G(f"xs{i}") for i in range(2)]
    cxs = [S.chan(f"xs{i}") for i in range(2)]
    zt = sb.alloc([128, 4, DM], BF16, "zt")
    gzt = G()
    czero = S.chan("zero")
    S.add("pool", lambda e: e.memset(zt, 0.0), writes=[gzt])
    for j in range(NT):
        S.add("sp", lambda e, j=j: e.dma_start(out=xg_d[j * 512:(j + 1) * 512, :].rearrange("(c p) d -> p c d", p=128), in_=zt), reads=[gzt], chan=czero)
    for tt in range(SEQ // 128):
        s = tt % 2
        S.add("sp", lambda e, tt=tt, s=s: e.dma_start(out=xs[s], in_=x_d[tt * 128:(tt + 1) * 128, :]), writes=[gxs[s]], chan=cxs[s])
        for b in range(2):
            bank = (tt % 2) * 2 + b

            def tr(e, s=s, b=b, bank=bank):
                ins = None
                for j in range(4):
                    kc = b * 4 + j
                    ins = e.transpose(psum[bank][:, j * 128:(j + 1) * 128], xs[s][:, kc * 128:(kc + 1) * 128], identf)
                return ins

            S.add("pe", tr, reads=[gxs[s], g_id], writes=[gps[bank]])
            dst = xT[:, b * 4:(b + 1) * 4, tt * 128:(tt + 1) * 128]
            src = psum[bank].rearrange("p (j t) -> p j t", j=4)
            if b == 0:
                S.add("act", lambda e, dst=dst, src=src: e.copy(out=dst, in_=src), reads=[gps[bank]], writes=xg(tt * 128, tt * 128 + 128))
            else:
                S.add("dve", lambda e, dst=dst, src=src: e.tensor_copy(out=dst, in_=src), reads=[gps[bank]], writes=xg(tt * 128, tt * 128 + 128))

    def dump_xT():
        if "xT" in dbg:
            S.barrier()
            sb.off = arena0
            tf = sb.alloc([128, NKC, 512], F32, "dbgt")
            gt = G("dbgt")
            for t in range(8):
                S.add("dve", lambda e, t=t: e.tensor_copy(out=tf, in_=xT[:, :, t * 512:(t + 1) * 512]), reads=[gxT[t]], writes=[gt])
                S.add("sp", lambda e, t=t: e.dma_start(out=dbg["xT"][:, :, t * 512:(t + 1) * 512], in_=tf), reads=[gt], chan=c_out)
            S.barrier()

    if stop == "P1":
        dump_xT()
        S.emit(nc)
        return nc

    def layer_body(layer):
        S.barrier()
        sb.regions = None
        sb.off = arena0
        wlu = [sb.alloc([128, NKC, 256], BF16, f"wlu{i}") for i in range(2)]
        gwlu = [G() for _ in range(2)]
        cwlu = [S.chan(f"wlu{i}") for i in range(2)]
        ufull = sb.alloc([128, 3 + SEQ], F32, "ufull")
        gu = [G() for _ in range(8)]
        gupad = G()
        ych = [sb.alloc([128, SEQ], BF16, f"ych{i}") for i in range(2)]
        gych = [G() for _ in range(2)]
        cych = [S.chan(f"ych{i}") for i in range(2)]
        NB = 3

        def bufs(name, dt=F32, n=NB, w=512):
            return [sb.alloc([128, w], dt, f"{name}{i}") for i in range(n)], [G(name) for _ in range(n)]

        gl, ggl = bufs("gl")
        uc, guc = bufs("uc")
        ucb, gucb = bufs("ucb", BF16)
        rr, grr = bufs("rr")
        ii, gii = bufs("ii")
        aa, gaa = bufs("aa")
        a2, ga2 = bufs("a2")
        bb, gbb = bufs("bb")
        hh, ghh = bufs("hh")
        S.add("pool", lambda e: e.memset(ufull[:, 0:3], 0.0), writes=[gupad])
        def mk_iter(c, tt, it):
            ws = c % 2
            pb = PP_LRU + (layer * 4 + c) * 8
            ys = c % 2
            s = it % NB
            sp_ = (it - 1) % NB
            t0 = tt * 512
            bu, bg = (0, 1) if tt % 2 == 0 else (2, 3)
            br, bi = (4, 5) if tt % 2 == 0 else (6, 7)
            ia = (layer * 2 + 0) * 4 + c
            ix = (layer * 2 + 1) * 4 + c
            cc = layer * 4 + c

            def stage_a():
                if tt == 0:
                    S.add("pool", lambda e: e.dma_start(out=wlu[ws][:, :, 0:128], in_=w_in_d[layer, 12 + c]), writes=[gwlu[ws]], chan=cwlu[ws])
                    S.add("pool", lambda e: e.dma_start(out=wlu[ws][:, :, 128:256], in_=w_in_d[layer, 16 + c]), writes=[gwlu[ws]], chan=cwlu[ws])

                def mm_u(e):
                    ins = None
                    for kc in range(NKC):
                        ins = e.matmul(psum[bu], lhsT=wlu[ws][:, kc, 0:128], rhs=xT[:, kc, t0:t0 + 512], start=(kc == 0), stop=(kc == NKC - 1))
                    return ins

                def mm_g(e):
                    ins = None
                    for kc in range(NKC):
                        ins = e.matmul(psum[bg], lhsT=wlu[ws][:, kc, 128:256], rhs=xT[:, kc, t0:t0 + 512], start=(kc == 0), stop=(kc == NKC - 1))
                    return ins

                S.add("pe", mm_u, reads=[gwlu[ws], gxT[tt]], writes=[gps[bu]])
                S.add("pe", mm_g, reads=[gwlu[ws], gxT[tt]], writes=[gps[bg]])
                S.add("dve", lambda e: e.tensor_copy(out=ufull[:, 3 + t0:3 + t0 + 512], in_=psum[bu]), reads=[gps[bu]], writes=[gu[tt]])
                S.add("act", lambda e: e.activation(out=gl[s], in_=psum[bg], func=AF.Gelu_apprx_tanh), reads=[gps[bg]], writes=[ggl[s]])
                rd = [gu[tt], gupad, g_const] + ([gu[tt - 1]] if tt > 0 else [])
                S.add("pool", lambda e: e.tensor_scalar(out=uc[s], in0=ufull[:, t0:t0 + 512], scalar1=pcol(pb), scalar2=pcol(pb + 4), op0=ALU.mult, op1=ALU.add), reads=rd, writes=[guc[s]])
                for j in range(1, 4):
                    S.add("dve", lambda e, j=j: e.scalar_tensor_tensor(out=uc[s], in0=ufull[:, t0 + j:t0 + j + 512], scalar=pcol(pb + j), in1=uc[s], op0=ALU.mult, op1=ALU.add), reads=rd + [guc[s]], writes=[guc[s]])
                S.add("act", lambda e: e.copy(out=ucb[s], in_=uc[s]), reads=[guc[s]], writes=[gucb[s]])

            def stage_b():
                S.add("pe", lambda e: e.matmul(psum[br], lhsT=wgate[:, ia, :], rhs=ucb[s], start=True, stop=True), reads=[gucb[s], g_const], writes=[gps[br]])
                S.add("pe", lambda e: e.matmul(psum[bi], lhsT=wgate[:, ix, :], rhs=ucb[s], start=True, stop=True), reads=[gucb[s], g_const], writes=[gps[bi]])
                S.add("act", lambda e: e.activation(out=rr[s], in_=psum[br], func=AF.Sigmoid, bias=pcol(pb + 5), scale=1.0), reads=[gps[br], g_const], writes=[grr[s]])
                S.add("act", lambda e: e.activation(out=ii[s], in_=psum[bi], func=AF.Sigmoid, bias=pcol(pb + 6), scale=1.0), reads=[gps[bi], g_const], writes=[gii[s]])
                S.add("act", lambda e: e.activation(out=aa[s], in_=rr[s], func=AF.Exp, scale=ppx[:, cc:cc + 1]), reads=[grr[s], g_ppx], writes=[gaa[s]])
                S.add("dve", lambda e: e.tensor_tensor(out=a2[s], in0=aa[s], in1=aa[s], op=ALU.mult), reads=[gaa[s]], writes=[ga2[s]])
                S.add("act", lambda e: e.activation(out=a2[s], in_=a2[s], func=AF.Sqrt, scale=-1.0, bias=1.0), reads=[ga2[s]], writes=[ga2[s]])
                S.add("dve", lambda e: e.tensor_tensor(out=bb[s], in0=ii[s], in1=uc[s], op=ALU.mult), reads=[gii[s], guc[s]], writes=[gbb[s]])
                S.add("dve", lambda e: e.tensor_tensor(out=bb[s], in0=bb[s], in1=a2[s], op=ALU.mult), reads=[gbb[s], ga2[s]], writes=[gbb[s]])

            def stage_c():
                if tt == 0:
                    S.add("dve", lambda e: e.tensor_tensor_scan(out=hh[s], data0=aa[s], data1=bb[s], initial=0.0, op0=ALU.mult, op1=ALU.add), reads=[gaa[s], gbb[s]], writes=[ghh[s]])
                else:
                    S.add("dve", lambda e: e.tensor_tensor_scan(out=hh[s], data0=aa[s], data1=bb[s], initial=hh[sp_][:, 511:512], op0=ALU.mult, op1=ALU.add), reads=[gaa[s], gbb[s], ghh[sp_]], writes=[ghh[s]])
                S.add("pool", lambda e: e.tensor_tensor(out=ych[ys][:, t0:t0 + 512], in0=gl[s], in1=hh[s], op=ALU.mult), reads=[ggl[s], ghh[s]], writes=[gych[ys]])
                if tt == 7:
                    S.add("sp", lambda e: e.dma_start(out=yscr[4 + c], in_=ych[ys]), reads=[gych[ys]], chan=cych[ys])

            return stage_a, stage_b, stage_c

        iters = [mk_iter(c, tt, c * 8 + tt) for c in range(4) for tt in range(8)]
        NI = len(iters)
        for k in range(NI + 2):
            if k < NI:
                iters[k][0]()
            if 0 <= k - 1 < NI:
                iters[k - 1][1]()
            if 0 <= k - 2 < NI:
                iters[k - 2][2]()

        if stop == f"LRU{layer}":
            return True
        S.barrier()
        sb.off = arena0
        tab = sb.alloc([128, 3, 8, 256], BF16, "tab")
        gtab = G()
        gtst = G()
        wq = [sb.alloc([128, NKC, 384], BF16, f"wq{i}") for i in range(2)]
        gwq = [G() for _ in range(2)]
        cwq = [S.chan(f"wq{i}") for i in range(2)]
        qT = sb.alloc([128, SEQ], BF16, "qT")
        kT = sb.alloc([128, SEQ], BF16, "kT")
        gq, gk = G(), G()
        vt = [sb.alloc([128, 32, 128], BF16, f"vt{i}") for i in range(2)]
        gvt = [G() for _ in range(2)]
        acc = sb.alloc([128, SEQ], F32, "acc")
        den = sb.alloc([128, SEQ], F32, "den")
        gacc, gden = G(), G()
        tstage = acc[:, 0:1024].rearrange("p (a b) -> p a b", a=4)
        vT = sb.alloc([128, SEQ], BF16, "vT")
        gv = G()
        pT = [sb.alloc([128, 256], BF16, f"pT{i}") for i in range(4)]
        gpT = [G() for _ in range(4)]
        ych = [sb.alloc([128, SEQ], BF16, "ycha0")] * 2
        gych = [G()] * 2
        cych = [S.chan("ycha0")] * 2
        ctab = S.chan("tab")
        for pi_ in range(3):
            for hq in range(2):
                S.add("sp", lambda e, pi_=pi_, hq=hq: e.dma_start(out=tstage, in_=tab_d[:, pi_, hq * 4:(hq + 1) * 4, :]), writes=[gtst], chan=ctab)
                S.add("act", lambda e, pi_=pi_, hq=hq: e.activation(out=tab[:, pi_, hq * 4:(hq + 1) * 4, :], in_=tstage, func=AF.Exp), reads=[gtst], writes=[gtab])
        vcount = 0
        pslot = 0
        GS = [G() for _ in range(4)]
        for hp in range(4):
            ws = hp % 2
            for j, gidx in enumerate((hp, 4 + hp, 8 + hp)):
                S.add("pool", lambda e, ws=ws, j=j, gidx=gidx: e.dma_start(out=wq[ws][:, :, j * 128:(j + 1) * 128], in_=w_in_d[layer, gidx]), writes=[gwq[ws]], chan=cwq[ws])
            for tt in range(8):
                t0 = tt * 512
                for which, (dst, gd) in enumerate(((qT, gq), (kT, gk), (vT, gv))):
                    bank = 6 + which % 2

                    def mmq(e, ws=ws, t0=t0, which=which, bank=bank):
                        ins = None
                        for kc in range(NKC):
                            ins = e.matmul(psum[bank], lhsT=wq[ws][:, kc, which * 128:(which + 1) * 128], rhs=xT[:, kc, t0:t0 + 512], start=(kc == 0), stop=(kc == NKC - 1))
                        return ins

                    S.add("pe", mmq, reads=[gwq[ws], gxT[tt]], writes=[gps[bank]])
                    if which == 0:
                        S.add("act", lambda e, t0=t0, bank=bank: e.activation(out=qT[:, t0:t0 + 512], in_=psum[bank], func=AF.Copy, scale=0.125), reads=[gps[bank]], writes=[gq])
                    elif which == 2:
                        S.add("act", lambda e, t0=t0, bank=bank: e.copy(out=vT[:, t0:t0 + 512], in_=psum[bank]), reads=[gps[bank]], writes=[gv])
                    else:
                        S.add("dve", lambda e, t0=t0, bank=bank: e.tensor_copy(out=kT[:, t0:t0 + 512], in_=psum[bank]), reads=[gps[bank]], writes=[gk])
            for pi, (window, dil) in enumerate(PATTERNS):
                nb = SEQ // (128 * dil)
                vs = vcount % 2
                vcount += 1
                for kb4 in range(8):
                    bank = 6 + (kb4 % 2)

                    def mmv(e, ws=ws, kb4=kb4, bank=bank, nb=nb, dil=dil):
                        ins = None
                        for j in range(4):
                            kb = kb4 * 4 + j
                            r, n = kb // nb, kb % nb
                            st = n * 128 * dil + r
                            ins = e.matmul(psum[bank][:, j * 128:(j + 1) * 128], lhsT=vT[:, st:st + 127 * dil + 1:dil], rhs=identb, start=True, stop=True)
                        return ins

                    S.add("pe", mmv, reads=[gv, g_const], writes=[gps[bank]])
                    eng = "act" if kb4 % 2 == 0 else "dve"
                    src = psum[bank].rearrange("p (j c) -> p j c", j=4)
                    dstv = vt[vs][:, kb4 * 4:(kb4 + 1) * 4, :]
                    if eng == "act":
                        S.add("act", lambda e, dstv=dstv, src=src: e.copy(out=dstv, in_=src), reads=[gps[bank]], writes=[gvt[vs]])
                    else:
                        S.add("dve", lambda e, dstv=dstv, src=src: e.tensor_copy(out=dstv, in_=src), reads=[gps[bank]], writes=[gvt[vs]])
                gs = min(4, nb)
                ogrp = 0
                fronts, backs = [], []
                for r in range(dil):
                    for n in range(nb):
                        kb = r * nb + n
                        st = n * 128 * dil + r
                        cnt = 256 if n < nb - 1 else 128
                        grp = n // gs
                        ob = 2 + (ogrp + grp) % 2
                        db = 4 + (ogrp + grp) % 2
                        col = (n % gs) * 128
                        nxt_grp = (n + 1) // gs
                        ob2 = 2 + (ogrp + nxt_grp) % 2
                        db2 = 4 + (ogrp + nxt_grp) % 2
                        col2 = ((n + 1) % gs) * 128
                        for h2 in range(2):
                            h = hp * 2 + h2
                            p0 = 64 * h2
                            sl = pslot % 4
                            pslot += 1
                            sbank = (0, 1, 6, 7)[sl]

                            def front(p0=p0, st=st, cnt=cnt, dil=dil, sbank=sbank, pi=pi, h=h, sl=sl):
                                def mms(e):
                                    return e.matmul(psum[sbank][:, 0:cnt], lhsT=kT[p0:p0 + 64, st:st + 127 * dil + 1:dil], rhs=qT[p0:p0 + 64, st:st + (cnt - 1) * dil + 1:dil], start=True, stop=True)

                                S.add("pe", mms, reads=[gq, gk], writes=[gps[sbank]])
                                S.add("act", lambda e: e.activation(out=pT[sl][:, 0:cnt], in_=psum[sbank][:, 0:cnt], func=AF.Exp), reads=[gps[sbank]], writes=[gpT[sl]])
                                S.add("dve", lambda e: e.tensor_tensor(out=pT[sl][:, 0:cnt], in0=pT[sl][:, 0:cnt], in1=tab[:, pi, h, 0:cnt], op=ALU.mult), reads=[gpT[sl], gtab], writes=[gpT[sl]])

                            def back(sl=sl, vs=vs, kb=kb, p0=p0, n=n, nb=nb, ob=ob, db=db, col=col, ob2=ob2, db2=db2, col2=col2, h2=h2, r=r, dil=dil, gs=gs, pi=pi):
                                def mmo(e):
                                    e.matmul(psum[ob][p0:p0 + 64, col:col + 128], lhsT=vt[vs][:, kb, p0:p0 + 64], rhs=pT[sl][:, 0:128], start=(n == 0), stop=True)
                                    ins = e.matmul(psum[db][p0:p0 + 64, col:col + 128], lhsT=onesb[:, 0:64], rhs=pT[sl][:, 0:128], start=(n == 0), stop=True)
                                    if n < nb - 1:
                                        e.matmul(psum[ob2][p0:p0 + 64, col2:col2 + 128], lhsT=vt[vs][:, kb, p0:p0 + 64], rhs=pT[sl][:, 128:256], start=True, stop=False)
                                        ins = e.matmul(psum[db2][p0:p0 + 64, col2:col2 + 128], lhsT=onesb[:, 0:64], rhs=pT[sl][:, 128:256], start=True, stop=False)
                                    return ins

                                wr = [gps[ob], gps[db]]
                                if n < nb - 1 and ob2 != ob:
                                    wr += [gps[ob2], gps[db2]]
                                S.add("pe", mmo, reads=[gpT[sl], gvt[vs], g_id], writes=wr)
                                if h2 == 1 and n % gs == gs - 1:
                                    g0 = n - (gs - 1)
                                    tst = g0 * 128 * dil + r
                                    ncol = gs * 128
                                    asl = acc[:, tst:tst + (ncol - 1) * dil + 1:dil]
                                    dsl = den[:, tst:tst + (ncol - 1) * dil + 1:dil]
                                    if pi == 0:
                                        S.add("dve", lambda e: e.tensor_copy(out=asl, in_=psum[ob][:, 0:ncol]), reads=[gps[ob]], writes=[gacc])
                                        S.add("dve", lambda e: e.tensor_copy(out=dsl, in_=psum[db][:, 0:ncol]), reads=[gps[db]], writes=[gden])
                                    else:
                                        S.add("dve", lambda e: e.tensor_tensor(out=asl, in0=asl, in1=psum[ob][:, 0:ncol], op=ALU.add), reads=[gps[ob], gacc], writes=[gacc])
                                        S.add("dve", lambda e: e.tensor_tensor(out=dsl, in0=dsl, in1=psum[db][:, 0:ncol], op=ALU.add), reads=[gps[db], gden], writes=[gden])

                            fronts.append(front)
                            backs.append(back)
                    ogrp += (nb + gs - 1) // gs
                LOOK = 3
                for i in range(len(fronts) + LOOK):
                    if i % 5 == 0:
                        conv_pull(1)
                    if i < len(fronts):
                        fronts[i]()
                    if i >= LOOK:
                        backs[i - LOOK]()
            ys = hp % 2
            S.add("dve", lambda e: e.reciprocal(out=den, in_=den), reads=[gden], writes=[gden])
            S.add("pool", lambda e, ys=ys: e.tensor_tensor(out=ych[ys], in0=acc, in1=den, op=ALU.mult), reads=[gacc, gden], writes=[gych[ys]])
            S.add("sp", lambda e, ys=ys, hp=hp: e.dma_start(out=yscr[hp], in_=ych[ys]), reads=[gych[ys]], chan=cych[ys])
        if stop == f"ATT{layer}":
            return True
        S.barrier()
        last = layer == NL - 1
        moe = layer % 2 == 1
        NP = SEQ // TP
        if layer == 0:
            cspill = S.chan("spill")
            for t in range(8):
                S.add("sp", lambda e, t=t: e.dma_start(out=xsA[:, :, t * 512:(t + 1) * 512], in_=xT[:, :, t * 512:(t + 1) * 512]), reads=[gxT[t]], chan=cspill)
            S.barrier()
        res_src = xsA if layer == 0 else xsB
        NTT = SEQ // 128
        if moe:
            assert last
            conv_pull(10 ** 6)
            S.barrier()
            sb.regions = None
            sb.off = xT_off
            x1tok = sb.alloc([128, NTT, DM], BF16, "x1tok")
            gx1tok = [G() for _ in range(NTT)]
            sb.off = cb_off
            lgall = sb.alloc([128, NTT, 8], F32, "lgall")
            glgall = G()
            w1 = sb.alloc([128, NTT], F32, "w1")
            w2 = sb.alloc([128, NTT], F32, "w2")
            slot1i = sb.alloc([128, NTT], mybir.dt.int32, "slot1i")
            slot2i = sb.alloc([128, NTT], mybir.dt.int32, "slot2i")
            widx = sb.alloc([128, NT, NGE], mybir.dt.int32, "widx")
            gw12, gslot, gwidx = G(), G(), G()
            arena1 = sb.off
        else:
            sb.regions = [(xT_off, xT_off + 128 * 0 + NKC * SEQ * 2), (cb_off, sb.limit)]
            sb.ridx = 0
            sb.off = xT_off
        accs = [sb.alloc([128, NKC, TP], F32, f"acc{i}") for i in range(2)]
        gaccs = [[G() for _ in range(NKC)] for _ in range(2)]
        if moe:
            x1bs = [sb.alloc([128, NKC, TP], BF16, "x1b0")] * 2
            gx1bs = [G()] * 2
        else:
            x1bs = [sb.alloc([128, NKC, TP], BF16, f"x1b{i}") for i in range(2)]
            gx1bs = [G() for _ in range(2)]
        ybuf = sb.alloc([128, NKC, TP], BF16, "ybuf")
        gybuf = G()
        cybuf = S.chan("ybuf")
        if moe:
            xres = [sb.alloc([128, NKC, TP], BF16, "xres0")] * 2
            gxres = [G()] * 2
            cxres = [S.chan("xres0")] * 2
        else:
            xres = [sb.alloc([128, NKC, TP], BF16, f"xres{i}") for i in range(2)]
            gxres = [G() for _ in range(2)]
            cxres = [S.chan(f"xres{i}") for i in range(2)]
        if not moe:
            wgs = [sb.alloc([128, NKC, W], BF16, f"wg{i}") for i in range(2)]
            wus = [sb.alloc([128, NKC, W], BF16, f"wu{i}") for i in range(2)]
            wds = [sb.alloc([128, GC, DM], BF16, f"wd{i}") for i in range(2)]
            gwgu = [G() for _ in range(2)]
            gwd = [G() for _ in range(2)]
            cgu = [S.chan(f"wgu{i}") for i in range(2)]
            cdd = [S.chan(f"wdd{i}") for i in range(2)]
            cguh = [S.chan(f"wguh{i}") for i in range(2)]
            cddh = [S.chan(f"wddh{i}") for i in range(2)]
            cwb = [S.chan(f"wwb{i}") for i in range(2)]
            cwbd = [S.chan(f"wwbd{i}") for i in range(2)]
            nst_ = DFF // W
            assert layer == 0
            wsc_g, wsc_u, wsc_d = wsc_g0, wsc_u0, wsc_d0
            gscr = [G() for _ in range(nst_)]
            gscrd = [G() for _ in range(nst_)]
            hT = [sb.alloc([128, GC, TP], BF16, f"hT{i}") for i in range(2)]
            ghT = [G() for _ in range(2)]
            sg = [sb.alloc([128, TP], F32, f"sg{i}") for i in range(2)]
            gsg = [G() for _ in range(2)]
        woutb = sb.alloc([128, NKC, DM], BF16, "woutb")
        gwout = G()
        wstage = [sb.alloc([128, DM], F32, f"wstage{i}") for i in range(2)]
        gwstage = [G() for _ in range(2)]
        cwstage = [S.chan(f"wstage{i}") for i in range(2)]
        sqb = sb.alloc([128, NKC, TP], BF16, "sqb")
        gsqb = G()
        ynb = sb.alloc([128, NKC, TP], BF16, "ynb")
        gynb = G()
        stt = [sb.alloc([128, TP], F32, f"stt{i}") for i in range(3)]
        gstt = [G() for _ in range(3)]
        if last:
            ostage = wstage
            gost = gwstage
            cost = [S.chan(f"ost{i}") for i in range(2)]
        else:
            xo = [sb.alloc([128, NKC, TP], BF16, "xo0")] * 2
            gxo = [G()] * 2
            cxo = [S.chan("xo0")] * 2
        for mc in range(NKC):
            s = mc % 2
            S.add("sp", lambda e, s=s, mc=mc: e.dma_start(out=wstage[s], in_=w_out_d[layer, :, mc, :]), writes=[gwstage[s]], chan=cwstage[s])
            S.add("dve", lambda e, s=s, mc=mc: e.tensor_scalar(out=woutb[:, mc, :], in0=wstage[s], scalar1=pcol(PP_GMIX + layer * 8 + mc), scalar2=None, op0=ALU.mult), reads=[gwstage[s], g_const], writes=[gwout])
        lnb = PP_LN + layer * 32
        bshape = [128, NKC, TP]

        def layer_norm(q, out_fn):
            accb, gacc = accs[q], gaccs[q]
            S.add("act", lambda e: e.copy(out=ynb, in_=accb), reads=gacc, writes=[gynb])
            S.add("pool", lambda e: e.tensor_tensor(out=sqb, in0=accb, in1=accb, op=ALU.mult), reads=gacc, writes=[gsqb])
            yield

            def mm1(e):
                ins = None
                for dc in range(NKC):
                    ins = e.matmul(psum[6], lhsT=onesb, rhs=ynb[:, dc, :], start=(dc == 0), stop=(dc == NKC - 1))
                return ins

            def mm2(e):
                ins = None
                for dc in range(NKC):
                    ins = e.matmul(psum[7], lhsT=onesb, rhs=sqb[:, dc, :], start=(dc == 0), stop=(dc == NKC - 1))
                return ins

            S.add("pe", mm1, reads=[gynb, g_id], writes=[gps[6]])
            S.add("pe", mm2, reads=[gsqb, g_id], writes=[gps[7]])
            yield
            mean, msq, var = stt[0], stt[1], stt[2]
            S.add("act", lambda e: e.activation(out=mean, in_=psum[6], func=AF.Copy, scale=1.0 / DM), reads=[gps[6]], writes=[gstt[0]])
            S.add("dve", lambda e: e.tensor_tensor(out=msq, in0=mean, in1=mean, op=ALU.mult), reads=[gstt[0]], writes=[gstt[1]])
            S.add("dve", lambda e: e.scalar_tensor_tensor(out=var, in0=psum[7], scalar=1.0 / DM, in1=msq, op0=ALU.mult, op1=ALU.subtract), reads=[gps[7], gstt[1]], writes=[gstt[2]])
            S.add("dve", lambda e: e.tensor_scalar(out=var, in0=var, scalar1=LN_EPS, scalar2=None, op0=ALU.add), reads=[gstt[2]], writes=[gstt[2]])
            S.add("act", lambda e: e.activation(out=var, in_=var, func=AF.Sqrt), reads=[gstt[2]], writes=[gstt[2]])
            S.add("dve", lambda e: e.reciprocal(out=var, in_=var), reads=[gstt[2]], writes=[gstt[2]])
            yield
            if moe:
                S.add("dve", lambda e: e.tensor_tensor(out=accb, in0=accb, in1=mean.unsqueeze(1).to_broadcast(bshape), op=ALU.subtract), reads=gacc + [gstt[0]], writes=gacc)
                S.add("dve", lambda e: e.tensor_tensor(out=accb, in0=accb, in1=var.unsqueeze(1).to_broadcast(bshape), op=ALU.mult), reads=gacc + [gstt[2]], writes=gacc)
                yield
            else:
                for dc in range(NKC):
                    S.add("dve", lambda e, dc=dc: e.tensor_tensor(out=accb[:, dc, :], in0=accb[:, dc, :], in1=mean, op=ALU.subtract), reads=[gacc[dc], gstt[0]], writes=[gacc[dc]])
                    S.add("dve", lambda e, dc=dc: e.tensor_tensor(out=accb[:, dc, :], in0=accb[:, dc, :], in1=var, op=ALU.mult), reads=[gacc[dc], gstt[2]], writes=[gacc[dc]])
                    if dc % 2 == 1:
                        yield
            for dc in range(NKC):
                out_fn(dc)
                if dc % 4 == 3:
                    yield

        def phase_a(p):
            q = p % 2
            t0 = p * TP
            accb, gacc, x1b, gx1b = accs[q], gaccs[q], x1bs[q], gx1bs[q]
            S.add("sp", lambda e: e.dma_start(out=xres[q], in_=res_src[:, :, t0:t0 + TP]), writes=[gxres[q]], chan=cxres[q])
            S.add("sp", lambda e: e.dma_start(out=ybuf, in_=yscr[:, :, t0:t0 + TP].rearrange("c p t -> p c t")), writes=[gybuf], chan=cybuf)
            S.add("act", lambda e: e.activation(out=sqb, in_=ybuf, func=AF.Square), reads=[gybuf], writes=[gsqb])
            yield
            for half in range(2):
                def mmss(e, half=half):
                    ins = None
                    for c in range(4):
                        ins = e.matmul(psum[6 + half], lhsT=onesb, rhs=sqb[:, half * 4 + c, :], start=(c == 0), stop=(c == 3))
                    return ins

                S.add("pe", mmss, reads=[gsqb, g_id], writes=[gps[6 + half]])
            yield
            for half in range(2):
                rs = stt[1 + half]
                grs = gstt[1 + half]
                S.add("dve", lambda e, rs=rs, half=half: e.tensor_scalar(out=rs, in0=psum[6 + half], scalar1=1.0 / 512, scalar2=RMS_EPS, op0=ALU.mult, op1=ALU.add), reads=[gps[6 + half]], writes=[grs])
                S.add("act", lambda e, rs=rs: e.activation(out=rs, in_=rs, func=AF.Sqrt), reads=[grs], writes=[grs])
                S.add("dve", lambda e, rs=rs: e.reciprocal(out=rs, in_=rs), reads=[grs], writes=[grs])
                S.add("pool", lambda e, rs=rs, half=half: e.tensor_tensor(out=ynb[:, half * 4:(half + 1) * 4, :], in0=ybuf[:, half * 4:(half + 1) * 4, :], in1=rs.unsqueeze(1).to_broadcast([128, 4, TP]), op=ALU.mult), reads=[gybuf, grs], writes=[gynb])
            yield
            for dc in range(NKC):
                bank = 6 + (dc % 2)

                def mmo_(e, dc=dc, bank=bank):
                    ins = None
                    for mc in range(NKC):
                        ins = e.matmul(psum[bank], lhsT=woutb[:, mc, dc * 128:(dc + 1) * 128], rhs=ynb[:, mc, :], start=(mc == 0), stop=(mc == NKC - 1))
                    return ins

                S.add("pe", mmo_, reads=[gynb, gwout], writes=[gps[bank]])
                S.add("dve", lambda e, dc=dc, bank=bank: e.scalar_tensor_tensor(out=accb[:, dc, :], in0=xres[q][:, dc, :], scalar=ALPHA, in1=psum[bank], op0=ALU.mult, op1=ALU.add), reads=[gps[bank], gxres[q]], writes=[gacc[dc]])
                if dc % 2 == 1:
                    yield

            def ln1_out(dc):
                S.add("act", lambda e, dc=dc: e.activation(out=x1b[:, dc, :], in_=accb[:, dc, :], func=AF.Identity, scale=pcol(lnb + dc), bias=pcol(lnb + 8 + dc)), reads=[gacc[dc], g_const], writes=[gx1b])
                S.add("pool", lambda e, dc=dc: e.tensor_scalar(out=accb[:, dc, :], in0=accb[:, dc, :], scalar1=ppx[:, 16 + layer * 16 + dc:16 + layer * 16 + dc + 1], scalar2=ppx[:, 16 + layer * 16 + 8 + dc:16 + layer * 16 + 8 + dc + 1], op0=ALU.mult, op1=ALU.add), reads=[gacc[dc], g_ppx], writes=[gacc[dc]])

            yield from layer_norm(q, ln1_out)
            if moe:
                def mmr(e):
                    ins = None
                    for tb in range(4):
                        for dc in range(NKC):
                            ins = e.matmul(psum[6][:, tb * 8:(tb + 1) * 8], lhsT=accb[:, dc, tb * 128:(tb + 1) * 128], rhs=routerb[:, dc, :], start=(dc == 0), stop=(dc == NKC - 1))
                    return ins

                S.add("pe", mmr, reads=gacc + [g_const], writes=[gps[6]])
                yield
                S.add("act", lambda e: e.activation(out=lgall[:, p * 4:(p + 1) * 4, :], in_=psum[6][:, 0:32].rearrange("p (a b) -> p a b", a=4), func=AF.Copy, scale=1.0 / ALPHA), reads=[gps[6]], writes=[glgall])
                for tb in range(4):
                    tt = p * 4 + tb
                    banks = (0, 1) if tb % 2 == 0 else (2, 3)
                    for half in range(2):
                        bank = banks[half]

                        def mmT(e, tb=tb, half=half, bank=bank):
                            ins = None
                            for j in range(4):
                                dc = half * 4 + j
                                ins = e.matmul(psum[bank][:, j * 128:(j + 1) * 128], lhsT=x1b[:, dc, tb * 128:(tb + 1) * 128], rhs=identb, start=True, stop=True)
                            return ins

                        S.add("pe", mmT, reads=[gx1b, g_const], writes=[gps[bank]])
                        dstt = x1tok[:, tt, half * 512:(half + 1) * 512]
                        if half == 0:
                            S.add("act", lambda e, dstt=dstt, bank=bank: e.copy(out=dstt, in_=psum[bank]), reads=[gps[bank]], writes=[gx1tok[tt]])
                        else:
                            S.add("dve", lambda e, dstt=dstt, bank=bank: e.tensor_copy(out=dstt, in_=psum[bank]), reads=[gps[bank]], writes=[gx1tok[tt]])
                    os_ = tb % 2

                    def mmtr(e, tb=tb):
                        ins = None
                        for dc in range(NKC):
                            ins = e.transpose(psum[4 + dc // 4][:, (dc % 4) * 128:(dc % 4 + 1) * 128], accb[:, dc, tb * 128:(tb + 1) * 128], identf)
                        return ins

                    S.add("pe", mmtr, reads=gacc + [g_id], writes=[gps[4], gps[5]])
                    S.add("act", lambda e, os_=os_: e.copy(out=ostage[os_][:, 0:512], in_=psum[4]), reads=[gps[4]], writes=[gost[os_]])
                    S.add("dve", lambda e, os_=os_: e.tensor_copy(out=ostage[os_][:, 512:1024], in_=psum[5]), reads=[gps[5]], writes=[gost[os_]])
                    S.add("sp", lambda e, os_=os_, tb=tb: e.dma_start(out=x1f_d[t0 + tb * 128:t0 + (tb + 1) * 128, :], in_=ostage[os_]), reads=[gost[os_]], chan=cost[os_])
                    yield

        def phase_b(p):
            q = p % 2
            t0 = p * TP
            accb, gacc = accs[q], gaccs[q]
            if not last:
                def ln2_out(dc):
                    S.add("act", lambda e, dc=dc: e.activation(out=xo[q][:, dc, :], in_=accb[:, dc, :], func=AF.Identity, scale=pcol(lnb + 16 + dc), bias=pcol(lnb + 24 + dc)), reads=[gacc[dc], g_const], writes=[gxo[q]])

                yield from layer_norm(q, ln2_out)
                S.add("sp", lambda e: e.dma_start(out=xsB[:, :, t0:t0 + TP], in_=xo[q]), reads=[gxo[q]], chan=cxo[q])
                yield
            else:
                def ln2_out(dc):
                    S.add("act", lambda e, dc=dc: e.activation(out=accb[:, dc, :], in_=accb[:, dc, :], func=AF.Identity, scale=pcol(lnb + 16 + dc), bias=pcol(lnb + 24 + dc)), reads=[gacc[dc], g_const], writes=[gacc[dc]])

                yield from layer_norm(q, ln2_out)
                for tb in range(TP // 128):
                    os_ = tb % 2

                    def mmtr(e, tb=tb):
                        ins = None
                        for dc in range(NKC):
                            ins = e.transpose(psum[6 + dc // 4][:, (dc % 4) * 128:(dc % 4 + 1) * 128], accb[:, dc, tb * 128:(tb + 1) * 128], identf)
                        return ins

                    S.add("pe", mmtr, reads=gacc + [g_id], writes=[gps[6], gps[7]])
                    yield
                    S.add("act", lambda e, os_=os_: e.copy(out=ostage[os_][:, 0:512], in_=psum[6]), reads=[gps[6]], writes=[gost[os_]])
                    S.add("dve", lambda e, os_=os_: e.tensor_copy(out=ostage[os_][:, 512:1024], in_=psum[7]), reads=[gps[7]], writes=[gost[os_]])
                    S.add("sp", lambda e, os_=os_, tb=tb: e.dma_start(out=out_d[t0 + tb * 128:t0 + (tb + 1) * 128, :], in_=ostage[os_]), reads=[gost[os_]], chan=cost[os_])
                    yield

        I32 = mybir.dt.int32

        def moe_routed():
            for p in range(NP):
                for _ in phase_a(p):
                    pass
            if stop == "M_C1":
                return
            S.barrier()
            sb.off = arena1
            sh3 = [128, NTT, 8]
            mx = sb.alloc(sh3, F32, "mx")
            m1 = sb.alloc(sh3, F32, "m1")
            m12f = sb.alloc([128, NTT * 8], F32, "m12")
            m12 = m12f.rearrange("p (a b) -> p a b", b=8)
            m2 = sb.alloc(sh3, F32, "m2")
            tots = sb.alloc(sh3, F32, "tots")
            tbase = sb.alloc(sh3, F32, "tbase")
            pos = sb.alloc(sh3, F32, "pos")
            pt = sb.alloc(sh3, F32, "pt")
            dd = sb.alloc([128, NTT], F32, "dd")
            e2 = sb.alloc([128, NTT], F32, "e2")
            s1f = sb.alloc([128, NTT], F32, "s1f")
            s2f = sb.alloc([128, NTT], F32, "s2f")
            cnt = sb.alloc([128, 8], F32, "cnt")
            pad = sb.alloc([128, 8], F32, "pad")
            t8 = sb.alloc([128, 8], F32, "t8")
            ends = sb.alloc([128, 8], F32, "ends")
            starts = sb.alloc([128, 8], F32, "starts")
            ej = sb.alloc([128, NT], F32, "ej")
            basej = sb.alloc([128, NT], F32, "basej")
            widxf = sb.alloc([128, NT, NGE], F32, "widxf")
            gmx, gm1, gm12, gm2, gtots, gtbase, gpos, gpt, gdd, ge2, gs1f, gs2f, gcnt, gpad, gt8, gends, gstarts, gej, gbasej, gwidxf = (G() for _ in range(20))
            for tt in range(NTT):
                S.add("dve", lambda e, tt=tt: e.max(out=mx[:, tt, :], in_=lgall[:, tt, :]), reads=[glgall], writes=[gmx])
            S.add("dve", lambda e: e.tensor_tensor(out=m1, in0=lgall, in1=mx[:, :, 0:1].to_broadcast(sh3), op=ALU.is_ge), reads=[glgall, gmx], writes=[gm1])
            S.add("dve", lambda e: e.tensor_tensor(out=m12, in0=lgall, in1=mx[:, :, 1:2].to_broadcast(sh3), op=ALU.is_ge), reads=[glgall, gmx], writes=[gm12])
            S.add("dve", lambda e: e.tensor_tensor(out=m2, in0=m12, in1=m1, op=ALU.subtract), reads=[gm12, gm1], writes=[gm2])
            S.add("dve", lambda e: e.tensor_tensor(out=dd, in0=mx[:, :, 1], in1=mx[:, :, 0], op=ALU.subtract), reads=[gmx], writes=[gdd])
            S.add("act", lambda e: e.activation(out=e2, in_=dd, func=AF.Exp), reads=[gdd], writes=[ge2])
            S.add("dve", lambda e: e.tensor_scalar(out=dd, in0=e2, scalar1=1.0, scalar2=None, op0=ALU.add), reads=[ge2], writes=[gdd])
            S.add("dve", lambda e: e.reciprocal(out=w1, in_=dd), reads=[gdd], writes=[gw12])
            S.add("dve", lambda e: e.tensor_tensor(out=w2, in0=e2, in1=w1, op=ALU.mult), reads=[ge2, gw12], writes=[gw12])
            S.add("pe", lambda e: e.matmul(psum[0][:, 0:NTT * 8], lhsT=ltri, rhs=m12f, start=True, stop=True), reads=[gm12, g_const], writes=[gps[0]])
            S.add("pe", lambda e: e.matmul(psum[1][:, 0:NTT * 8], lhsT=onesf, rhs=m12f, start=True, stop=True), reads=[gm12, g_id], writes=[gps[1]])
            S.add("dve", lambda e: e.tensor_copy(out=tots, in_=psum[1][:, 0:NTT * 8].rearrange("p (a b) -> p a b", b=8)), reads=[gps[1]], writes=[gtots])
            S.add("pool", lambda e: e.memset(tbase[:, 0, :], 0.0), writes=[gtbase])
            for tt in range(1, NTT):
                S.add("dve", lambda e, tt=tt: e.tensor_tensor(out=tbase[:, tt, :], in0=tbase[:, tt - 1, :], in1=tots[:, tt - 1, :], op=ALU.add), reads=[gtots, gtbase], writes=[gtbase])
            S.add("dve", lambda e: e.tensor_tensor(out=cnt, in0=tbase[:, NTT - 1, :], in1=tots[:, NTT - 1, :], op=ALU.add), reads=[gtots, gtbase], writes=[gcnt])
            S.add("dve", lambda e: e.tensor_scalar(out=pad, in0=cnt, scalar1=0.0, scalar2=512.0, op0=ALU.is_gt, op1=ALU.mult), reads=[gcnt], writes=[gpad])
            for k in range(1, 8):
                S.add("dve", lambda e, k=k: e.tensor_scalar(out=t8, in0=cnt, scalar1=512.0 * k, scalar2=512.0, op0=ALU.is_gt, op1=ALU.mult), reads=[gcnt], writes=[gt8])
                S.add("dve", lambda e: e.tensor_tensor(out=pad, in0=pad, in1=t8, op=ALU.add), reads=[gt8, gpad], writes=[gpad])
            S.add("dve", lambda e: e.tensor_copy(out=ends[:, 0:1], in_=pad[:, 0:1]), reads=[gpad], writes=[gends])
            for ex in range(1, 8):
                S.add("dve", lambda e, ex=ex: e.tensor_tensor(out=ends[:, ex:ex + 1], in0=ends[:, ex - 1:ex], in1=pad[:, ex:ex + 1], op=ALU.add), reads=[gpad, gends], writes=[gends])
            S.add("dve", lambda e: e.tensor_tensor(out=starts, in0=ends, in1=pad, op=ALU.subtract), reads=[gpad, gends], writes=[gstarts])
            S.add("dve", lambda e: e.tensor_tensor(out=pos, in0=tbase, in1=psum[0][:, 0:NTT * 8].rearrange("p (a b) -> p a b", b=8), op=ALU.add), reads=[gps[0], gtbase], writes=[gpos])
            S.add("dve", lambda e: e.tensor_tensor(out=pos, in0=pos, in1=starts.unsqueeze(1).to_broadcast(sh3), op=ALU.add), reads=[gpos, gstarts], writes=[gpos])
            S.add("dve", lambda e: e.tensor_tensor(out=pt, in0=pos, in1=m1, op=ALU.mult), reads=[gpos, gm1], writes=[gpt])
            S.add("dve", lambda e: e.reduce_sum(out=s1f, in_=pt, axis=AX.X), reads=[gpt], writes=[gs1f])
            S.add("dve", lambda e: e.tensor_tensor(out=pt, in0=pos, in1=m2, op=ALU.mult), reads=[gpos, gm2, gs1f], writes=[gpt])
            S.add("dve", lambda e: e.reduce_sum(out=s2f, in_=pt, axis=AX.X), reads=[gpt], writes=[gs2f])
            S.add("dve", lambda e: e.tensor_copy(out=slot1i, in_=s1f), reads=[gs1f], writes=[gslot])
            S.add("dve", lambda e: e.tensor_copy(out=slot2i, in_=s2f), reads=[gs2f], writes=[gslot])
            for j in range(NT):
                S.add("dve", lambda e, j=j: e.tensor_scalar(out=t8, in0=ends, scalar1=512.0 * j, scalar2=None, op0=ALU.is_le), reads=[gends], writes=[gt8])
                S.add("dve", lambda e, j=j: e.reduce_sum(out=ej[:, j:j + 1], in_=t8, axis=AX.X), reads=[gt8], writes=[gej])
            S.add("dve", lambda e: e.tensor_scalar(out=ej, in0=ej, scalar1=float(NEXP - 1), scalar2=float(NGE * 128), op0=ALU.min, op1=ALU.mult), reads=[gej], writes=[gej])
            S.add("dve", lambda e: e.tensor_tensor(out=basej, in0=ej, in1=iotap.to_broadcast([128, NT]), op=ALU.add), reads=[gej, g_const], writes=[gbasej])
            for gi in range(NGE):
                S.add("dve", lambda e, gi=gi: e.tensor_scalar(out=widxf[:, :, gi], in0=basej, scalar1=128.0 * gi, scalar2=None, op0=ALU.add), reads=[gbasej], writes=[gwidxf])
            S.add("dve", lambda e: e.tensor_copy(out=widx, in_=widxf), reads=[gwidxf], writes=[gwidx])
            cscat = [S.chan(f"scat{i}") for i in range(2)]
            for tt in range(NTT):
                for k, sl in enumerate((slot1i, slot2i)):
                    S.add("pool", lambda e, tt=tt, sl=sl: e.indirect_dma_start(out=xg_d[:, :], out_offset=bass.IndirectOffsetOnAxis(ap=sl[:, tt:tt + 1], axis=0), in_=x1tok[:, tt, :], in_offset=None), reads=[gx1tok[tt], gslot], chan=cscat[k])
            S.barrier()
            if stop == "M_RT":
                return
            sb.off = xT_off
            NW = 4
            wgs = [sb.alloc([128, NKC * W], BF16, f"ewg{i}") for i in range(NW)]
            wus = [sb.alloc([128, NKC * W], BF16, f"ewu{i}") for i in range(NW)]
            wds = [sb.alloc([128, GC * DM], BF16, f"ewd{i}") for i in range(NW)]
            wgs3 = [t.rearrange("p (a b) -> p a b", a=NKC) for t in wgs]
            wus3 = [t.rearrange("p (a b) -> p a b", a=NKC) for t in wus]
            wds3 = [t.rearrange("p (a b) -> p a b", a=GC) for t in wds]
            gwgu = [G() for _ in range(NW)]
            gwd = [G() for _ in range(NW)]
            cgu = [S.chan(f"ewgu{i}") for i in range(NW)]
            cdd = [S.chan(f"ewdd{i}") for i in range(NW)]
            xgt = [sb.alloc([128, 4, DM], BF16, f"xgt{i}") for i in range(2)]
            gxgt = [G() for _ in range(2)]
            cxgt = [S.chan(f"xgt{i}") for i in range(2)]
            assert sb.off <= xT_off + NKC * SEQ * 2
            sb.off = arena1
            xe = [sb.alloc([128, NKC, 512], BF16, f"xe{i}") for i in range(2)]
            gxe = [G() for _ in range(2)]
            yacc = [sb.alloc([128, 4, DM], F32, f"yacc{i}") for i in range(2)]
            gy = [[G() for _ in range(8)] for _ in range(2)]
            cy = [S.chan(f"yst{i}") for i in range(2)]
            hT = [sb.alloc([128, GC, 512], BF16, f"ehT{i}") for i in range(2)]
            ghT = [G() for _ in range(2)]
            sg = [sb.alloc([128, 512], F32, f"esg{i}") for i in range(2)]
            gsg = [G() for _ in range(2)]
            NS = NT * NGE

            def prep(j):
                b = j % 2
                S.add("sp", lambda e: e.dma_start(out=xgt[b], in_=xg_d[j * 512:(j + 1) * 512, :].rearrange("(c p) d -> p c d", p=128)), writes=[gxgt[b]], chan=cxgt[b])
                for dc in range(NKC):
                    bank = 6 + dc % 2

                    def mm(e, dc=dc, bank=bank):
                        ins = None
                        for c in range(4):
                            ins = e.matmul(psum[bank][:, c * 128:(c + 1) * 128], lhsT=xgt[b][:, c, dc * 128:(dc + 1) * 128], rhs=identb, start=True, stop=True)
                        return ins

                    S.add("pe", mm, reads=[gxgt[b], g_const], writes=[gps[bank]])
                    if dc % 2 == 0:
                        S.add("act", lambda e, dc=dc, bank=bank: e.copy(out=xe[b][:, dc, :], in_=psum[bank]), reads=[gps[bank]], writes=[gxe[b]])
                    else:
                        S.add("dve", lambda e, dc=dc, bank=bank: e.tensor_copy(out=xe[b][:, dc, :], in_=psum[bank]), reads=[gps[bank]], writes=[gxe[b]])

            def load_w(i, part):
                j, gi = divmod(i, NGE)
                s = i % NW
                off = bass.IndirectOffsetOnAxis(ap=widx[:, j, gi:gi + 1], axis=0)
                if part == 0:
                    S.add("pool", lambda e: e.indirect_dma_start(out=wgs[s], out_offset=None, in_=wsc_gm[:, :], in_offset=off), reads=[gwidx], writes=[gwgu[s]], chan=cgu[s])
                    S.add("pool", lambda e: e.indirect_dma_start(out=wus[s], out_offset=None, in_=wsc_um[:, :], in_offset=off), reads=[gwidx], writes=[gwgu[s]], chan=cgu[s])
                else:
                    S.add("pool", lambda e: e.indirect_dma_start(out=wds[s], out_offset=None, in_=wsc_dm[:, :], in_offset=off), reads=[gwidx], writes=[gwd[s]], chan=cdd[s])

            def gate_up(i):
                j, gi = divmod(i, NGE)
                s = i % NW
                hs = i % 2
                b = j % 2
                for fc in range(GC):
                    k = fc % 2
                    bg_, bu_ = k, 2 + k

                    def mmg(e, fc=fc, bg_=bg_):
                        ins = None
                        for kc in range(NKC):
                            ins = e.matmul(psum[bg_], lhsT=wgs3[s][:, kc, fc * 128:(fc + 1) * 128], rhs=xe[b][:, kc, :], start=(kc == 0), stop=(kc == NKC - 1))
                        return ins

                    def mmu(e, fc=fc, bu_=bu_):
                        ins = None
                        for kc in range(NKC):
                            ins = e.matmul(psum[bu_], lhsT=wus3[s][:, kc, fc * 128:(fc + 1) * 128], rhs=xe[b][:, kc, :], start=(kc == 0), stop=(kc == NKC - 1))
                        return ins

                    S.add("pe", mmg, reads=[gwgu[s], gxe[b]], writes=[gps[bg_]])
                    S.add("pe", mmu, reads=[gwgu[s], gxe[b]], writes=[gps[bu_]])
                    S.add("act", lambda e, k=k, bg_=bg_: e.activation(out=sg[k], in_=psum[bg_], func=AF.Silu), reads=[gps[bg_]], writes=[gsg[k]])
                    S.add("dve", lambda e, fc=fc, k=k, bu_=bu_: e.tensor_tensor(out=hT[hs][:, fc, :], in0=sg[k], in1=psum[bu_], op=ALU.mult), reads=[gsg[k], gps[bu_]], writes=[ghT[hs]])

            def down(i):
                j, gi = divmod(i, NGE)
                s = i % NW
                hs = i % 2
                b = j % 2
                for c in range(4):
                    for half in range(2):
                        bd = 4 + half
                        gq_ = gy[b][c * 2 + half]

                        def mmd(e, c=c, half=half, bd=bd):
                            ins = None
                            for fc in range(GC):
                                ins = e.matmul(psum[bd], lhsT=hT[hs][:, fc, c * 128:(c + 1) * 128], rhs=wds3[s][:, fc, half * 512:(half + 1) * 512], start=(fc == 0), stop=(fc == GC - 1))
                            return ins

                        S.add("pe", mmd, reads=[gwd[s], ghT[hs]], writes=[gps[bd]])
                        dst = yacc[b][:, c, half * 512:(half + 1) * 512]
                        if gi == 0:
                            S.add("dve", lambda e, dst=dst, bd=bd: e.tensor_copy(out=dst, in_=psum[bd]), reads=[gps[bd]], writes=[gq_])
                        else:
                            S.add("dve", lambda e, dst=dst, bd=bd: e.tensor_tensor(out=dst, in0=dst, in1=psum[bd], op=ALU.add), reads=[gps[bd], gq_], writes=[gq_])
                if gi == NGE - 1:
                    S.add("sp", lambda e: e.dma_start(out=yd_d[j * 512:(j + 1) * 512, :].rearrange("(c p) d -> p c d", p=128), in_=yacc[b]), reads=gy[b], chan=cy[b])

            prep(0)
            for i in range(min(NW - 1, NS)):
                load_w(i, 0)
                load_w(i, 1)
            for i in range(NS):
                j, gi = divmod(i, NGE)
                if i + NW - 1 < NS:
                    load_w(i + NW - 1, 0)
                if gi == 6 and j + 1 < NT:
                    prep(j + 1)
                gate_up(i)
                if i > 0:
                    down(i - 1)
                if i + NW - 1 < NS:
                    load_w(i + NW - 1, 1)
            down(NS - 1)
            S.barrier()
            if stop == "M_EX":
                return
            sb.off = xT_off
            gb = sb.alloc([128, 2, DM], F32, "gb")
            ggb = G()
            cgb = S.chan("gb")
            S.add("sp", lambda e: e.dma_start(out=gb, in_=ln2row_d), writes=[ggb], chan=cgb)
            NBC = 4
            sb.off = arena1
            y1 = [sb.alloc([128, DM], F32, f"y1_{i}") for i in range(NBC)]
            y2 = [sb.alloc([128, DM], F32, f"y2_{i}") for i in range(NBC)]
            xr = [sb.alloc([128, DM], F32, f"xr{i}") for i in range(NBC)]
            zq = [sb.alloc([128, DM], F32, f"zq{i}") for i in range(NBC)]
            stc = [sb.alloc([128, 8], F32, f"stc{i}") for i in range(NBC)]
            gy1 = [G() for _ in range(NBC)]
            gy2 = [G() for _ in range(NBC)]
            gxr = [G() for _ in range(NBC)]
            gzq = [G() for _ in range(NBC)]
            gstc = [G() for _ in range(NBC)]
            cg1 = [S.chan(f"cg1_{i}") for i in range(NBC)]
            cg2 = [S.chan(f"cg2_{i}") for i in range(NBC)]
            cxr = [S.chan(f"cxr{i}") for i in range(NBC)]
            cfo = [S.chan(f"cfo{i}") for i in range(NBC)]
            for tt in range(NTT):
                b = tt % NBC
                st = stc[b]
                S.add("pool", lambda e, tt=tt, b=b: e.indirect_dma_start(out=y1[b], out_offset=None, in_=yd_d[:, :], in_offset=bass.IndirectOffsetOnAxis(ap=slot1i[:, tt:tt + 1], axis=0)), reads=[gslot], writes=[gy1[b]], chan=cg1[b])
                S.add("pool", lambda e, tt=tt, b=b: e.indirect_dma_start(out=y2[b], out_offset=None, in_=yd_d[:, :], in_offset=bass.IndirectOffsetOnAxis(ap=slot2i[:, tt:tt + 1], axis=0)), reads=[gslot], writes=[gy2[b]], chan=cg2[b])
                S.add("sp", lambda e, tt=tt, b=b: e.dma_start(out=xr[b], in_=x1f_d[tt * 128:(tt + 1) * 128, :]), writes=[gxr[b]], chan=cxr[b])
                S.add("dve", lambda e, tt=tt, b=b: e.scalar_tensor_tensor(out=xr[b], in0=y1[b], scalar=w1[:, tt:tt + 1], in1=xr[b], op0=ALU.mult, op1=ALU.add), reads=[gy1[b], gxr[b], gw12], writes=[gxr[b]])
                S.add("dve", lambda e, tt=tt, b=b: e.scalar_tensor_tensor(out=xr[b], in0=y2[b], scalar=w2[:, tt:tt + 1], in1=xr[b], op0=ALU.mult, op1=ALU.add), reads=[gy2[b], gxr[b], gw12], writes=[gxr[b]])
                S.add("act", lambda e, b=b: e.activation(out=zq[b], in_=xr[b], func=AF.Square), reads=[gxr[b]], writes=[gzq[b]])
                S.add("dve", lambda e, b=b, st=st: e.reduce_sum(out=st[:, 0:1], in_=xr[b], axis=AX.X), reads=[gxr[b]], writes=[gstc[b]])
                S.add("dve", lambda e, b=b, st=st: e.reduce_sum(out=st[:, 1:2], in_=zq[b], axis=AX.X), reads=[gzq[b]], writes=[gstc[b]])
                S.add("dve", lambda e, st=st: e.tensor_scalar(out=st[:, 2:3], in0=st[:, 0:1], scalar1=1.0 / DM, scalar2=None, op0=ALU.mult), reads=[gstc[b]], writes=[gstc[b]])
                S.add("dve", lambda e, st=st: e.tensor_tensor(out=st[:, 3:4], in0=st[:, 2:3], in1=st[:, 2:3], op=ALU.mult), reads=[gstc[b]], writes=[gstc[b]])
                S.add("dve", lambda e, st=st: e.scalar_tensor_tensor(out=st[:, 4:5], in0=st[:, 1:2], scalar=1.0 / DM, in1=st[:, 3:4], op0=ALU.mult, op1=ALU.subtract), reads=[gstc[b]], writes=[gstc[b]])
                S.add("dve", lambda e, st=st: e.tensor_scalar(out=st[:, 4:5], in0=st[:, 4:5], scalar1=LN_EPS, scalar2=None, op0=ALU.add), reads=[gstc[b]], writes=[gstc[b]])
                S.add("act", lambda e, st=st: e.activation(out=st[:, 4:5], in_=st[:, 4:5], func=AF.Sqrt), reads=[gstc[b]], writes=[gstc[b]])
                S.add("dve", lambda e, st=st: e.reciprocal(out=st[:, 5:6], in_=st[:, 4:5]), reads=[gstc[b]], writes=[gstc[b]])
                S.add("dve", lambda e, b=b, st=st: e.scalar_tensor_tensor(out=zq[b], in0=xr[b], scalar=st[:, 2:3], in1=gb[:, 0, :], op0=ALU.subtract, op1=ALU.mult), reads=[gxr[b], gstc[b], gzq[b], ggb], writes=[gzq[b]])
                S.add("dve", lambda e, b=b, st=st: e.scalar_tensor_tensor(out=zq[b], in0=zq[b], scalar=st[:, 5:6], in1=gb[:, 1, :], op0=ALU.mult, op1=ALU.add), reads=[gzq[b], gstc[b], ggb], writes=[gzq[b]])
                S.add("sp", lambda e, tt=tt, b=b: e.dma_start(out=out_d[tt * 128:(tt + 1) * 128, :], in_=zq[b]), reads=[gzq[b]], chan=cfo[b])

        nexp = NEXP if moe else 1
        ng = (EFF if moe else DFF) // W
        stages = [(ex, gi) for ex in range(nexp) for gi in range(ng)]

        def ffn(p, inter, per_stage):
            q = p % 2
            accb, gacc, x1b, gx1b = accs[q], gaccs[q], x1bs[q], gx1bs[q]
            xup = x1c if moe else x1b
            gxup = gx1c if moe else gx1b

            def load_w(i, part):
                ex, gi = stages[i]
                s = i % 2
                if p == 0:
                    if moe:
                        srcs = (mg_d[ex, gi], mu_d[ex, gi], md_d[ex, gi])
                    else:
                        srcs = (fg_d[gi], fu_d[gi], fd_d[gi])
                    if part == 0:
                        S.add("pool", lambda e, s=s, src=srcs[0]: e.dma_start(out=wgs[s], in_=src), writes=[gwgu[s]], chan=cgu[s])
                        S.add("pool", lambda e, s=s, src=srcs[1]: e.dma_start(out=wus[s], in_=src), writes=[gwgu[s]], chan=cgu[s])
                        S.add("sp", lambda e, s=s, i=i: e.dma_start(out=wsc_g[i], in_=wgs[s]), reads=[gwgu[s]], writes=[gscr[i]], chan=cwb[s])
                        S.add("sp", lambda e, s=s, i=i: e.dma_start(out=wsc_u[i], in_=wus[s]), reads=[gwgu[s]], writes=[gscr[i]], chan=cwb[s])
                    else:
                        S.add("pool", lambda e, s=s, src=srcs[2]: e.dma_start(out=wds[s], in_=src), writes=[gwd[s]], chan=cdd[s])
                        S.add("sp", lambda e, s=s, i=i: e.dma_start(out=wsc_d[i], in_=wds[s]), reads=[gwd[s]], writes=[gscrd[i]], chan=cwbd[s])
                else:
                    if part == 0:
                        S.add("sp", lambda e, s=s, i=i: e.dma_start(out=wgs[s], in_=wsc_g[i]), reads=[gscr[i]], writes=[gwgu[s]], chan=cguh[s])
                        S.add("sp", lambda e, s=s, i=i: e.dma_start(out=wus[s], in_=wsc_u[i]), reads=[gscr[i]], writes=[gwgu[s]], chan=cguh[s])
                    else:
                        S.add("sp", lambda e, s=s, i=i: e.dma_start(out=wds[s], in_=wsc_d[i]), reads=[gscrd[i]], writes=[gwd[s]], chan=cddh[s])

            def gate_up(i):
                s = i % 2
                for fc in range(GC):
                    k = fc % 2
                    bg_, bu_ = k, 2 + k

                    def mmg(e, s=s, fc=fc, bg_=bg_):
                        ins = None
                        for kc in range(NKC):
                            ins = e.matmul(psum[bg_], lhsT=wgs[s][:, kc, fc * 128:(fc + 1) * 128], rhs=x1b[:, kc, :], start=(kc == 0), stop=(kc == NKC - 1))
                        return ins

                    def mmu(e, s=s, fc=fc, bu_=bu_):
                        ins = None
                        for kc in range(NKC):
                            ins = e.matmul(psum[bu_], lhsT=wus[s][:, kc, fc * 128:(fc + 1) * 128], rhs=xup[:, kc, :], start=(kc == 0), stop=(kc == NKC - 1))
                        return ins

                    S.add("pe", mmg, reads=[gwgu[s], gx1b], writes=[gps[bg_]])
                    S.add("pe", mmu, reads=[gwgu[s], gxup], writes=[gps[bu_]])
                    S.add("act", lambda e, k=k, bg_=bg_: e.activation(out=sg[k], in_=psum[bg_], func=AF.Silu), reads=[gps[bg_]], writes=[gsg[k]])
                    S.add("dve", lambda e, s=s, fc=fc, k=k, bu_=bu_: e.tensor_tensor(out=hT[s][:, fc, :], in0=sg[k], in1=psum[bu_], op=ALU.mult), reads=[gsg[k], gps[bu_]], writes=[ghT[s]])
                    pull(1)

            def down(i):
                s = i % 2
                for dc in range(NKC):
                    bd = 4 + dc % 2

                    def mmd(e, s=s, dc=dc, bd=bd):
                        ins = None
                        for fc in range(GC):
                            ins = e.matmul(psum[bd], lhsT=wds[s][:, fc, dc * 128:(dc + 1) * 128], rhs=hT[s][:, fc, :], start=(fc == 0), stop=(fc == GC - 1))
                        return ins

                    S.add("pe", mmd, reads=[gwd[s], ghT[s]], writes=[gps[bd]])
                    S.add("dve", lambda e, dc=dc, bd=bd: e.tensor_tensor(out=accb[:, dc, :], in0=accb[:, dc, :], in1=psum[bd], op=ALU.add), reads=[gps[bd], gacc[dc]], writes=[gacc[dc]])

            def pull(n):
                for _ in range(n):
                    if next(inter, "done") == "done":
                        return

            load_w(0, 0)
            load_w(0, 1)
            for i, (ex, gi) in enumerate(stages):
                if i + 1 < len(stages):
                    load_w(i + 1, 0)
                if moe and gi == 0:
                    S.add("pe", lambda e, ex=ex: e.matmul(psum[5], lhsT=selb[:, ex, :], rhs=combTs[q], start=True, stop=True), reads=[gcombTs[q], g_const], writes=[gps[5]])
                    for kc in range(NKC):
                        S.add("dve", lambda e, kc=kc: e.tensor_tensor(out=x1c[:, kc, :], in0=x1b[:, kc, :], in1=psum[5], op=ALU.mult), reads=[gps[5], gx1b], writes=[gx1c])
                gate_up(i)
                if i > 0:
                    down(i - 1)
                if i + 1 < len(stages):
                    load_w(i + 1, 1)
                pull(max(per_stage - GC, 0))
            down(len(stages) - 1)
            pull(10 ** 6)

        def chain_(*gens):
            for g_ in gens:
                yield from g_

        if moe:
            moe_routed()
            return False
        for _ in phase_a(0):
            pass
        per_stage = 1 if moe else 3
        for p in range(NP):
            gens = []
            if p > 0:
                gens.append(phase_b(p - 1))
            if p + 1 < NP:
                gens.append(phase_a(p + 1))
            ffn(p, chain_(*gens), per_stage)
        for _ in phase_b(NP - 1):
            pass
        if not last:
            S.barrier()
            crl = S.chan("reload")
            for t in range(8):
                S.add("sp", lambda e, t=t: e.dma_start(out=xT[:, :, t * 512:(t + 1) * 512], in_=xsB[:, :, t * 512:(t + 1) * 512]), writes=[gxT[t]], chan=crl)
        if stop == f"C{layer}":
            return True
        return False

    for layer_i in range(NL):
        if layer_body(layer_i):
            break
    S.barrier()
    if "xT" in dbg:
        dump_xT()
    if "y" in dbg:
        sb.off = arena0
        tb = sb.alloc([128, SEQ], BF16, "dbgy")
        tf = sb.alloc([128, SEQ], F32, "dbgyf")
        g1, g2 = G(), G()
        cd = S.chan("dbgy")
        for c in range(8):
            S.add("sp", lambda e, c=c: e.dma_start(out=tb, in_=yscr[c]), writes=[g1], chan=cd)
            S.add("dve", lambda e: e.tensor_copy(out=tf, in_=tb), reads=[g1], writes=[g2])
            S.add("sp", lambda e, c=c: e.dma_start(out=dbg["y"][c], in_=tf), reads=[g2], chan=c_out)
    S.emit(nc)
    return nc


_CACHE = {}


def kernel(**inputs):
    sh = _prep_shared(inputs)
    x = np.ascontiguousarray(inputs["x"], dtype=np.float32)
    if "nc" not in _CACHE:
        _CACHE["nc"] = build_program()
    nc = _CACHE["nc"]
    in_maps = []
    for b in range(x.shape[0]):
        m = dict(sh)
        m["x"] = np.ascontiguousarray(x[b])
        in_maps.append(m)
    res = run_bass_kernel_spmd(nc, in_maps, core_ids=list(range(x.shape[0])))
    return np.stack([np.asarray(r["out"]) for r in res.results], axis=0).astype(np.float32)
```
